# Optimizing a Trainium2 kernel written in Bass

```python
import jax, jax.numpy as jnp
from jax import lax
import numpy as np

D_MODEL = 1024
BATCH = 2
SEQ = 8192
DEPTH = 2

MIX_WIDTH = D_MODEL
ATT_HEADS = 8
ATT_HEAD_DIM = 64
ATT_W = ATT_HEADS * ATT_HEAD_DIM
ROPE_DIM = ATT_HEAD_DIM // 4
ROPE_THETA = 500000.0
MOBA_BLOCK = 256
MOBA_TOPK = 3
Q_BLOCK = 64
MLSTM_HEADS = 4
MLSTM_QK_DIM = 64
MLSTM_V_DIM = 128
ML_QK_W = MLSTM_HEADS * MLSTM_QK_DIM
ML_V_W = MLSTM_HEADS * MLSTM_V_DIM
MLSTM_CHUNK = 64
D_FF = 4 * D_MODEL
EPS = 1e-6
IN_SPLITS = (ATT_W, ATT_W, ATT_W, ML_QK_W, ML_QK_W, ML_V_W, ML_V_W, MLSTM_HEADS, MLSTM_HEADS)
IN_WIDTH = 3 * ATT_W + 2 * ML_QK_W + 2 * ML_V_W + 2 * MLSTM_HEADS

kernel_name = "hybrid_moba_mlstm_parallel_heads"


def rms_norm(x, g):
    xf = x.astype(jnp.float32)
    y = xf * lax.rsqrt(jnp.mean(xf * xf, axis=-1, keepdims=True) + EPS) * g.astype(jnp.float32)
    return y.astype(x.dtype)


def partial_rope(x, positions):
    inv_freq = ROPE_THETA ** (-jnp.arange(0, ROPE_DIM, 2, dtype=jnp.float32) / ROPE_DIM)
    ang = positions.astype(jnp.float32)[:, None, :, None] * inv_freq
    cos = jnp.cos(jnp.concatenate([ang, ang], axis=-1))
    sin = jnp.sin(jnp.concatenate([ang, ang], axis=-1))
    xr, xp = x[..., :ROPE_DIM], x[..., ROPE_DIM:]
    x1, x2 = xr[..., :ROPE_DIM // 2], xr[..., ROPE_DIM // 2:]
    xr = xr * cos + jnp.concatenate([-x2, x1], axis=-1) * sin
    return jnp.concatenate([xr, xp], axis=-1)


def moba_attention(q, k, v):
    B, H, S, Dh = q.shape
    nb = -(-S // MOBA_BLOCK)
    pad = nb * MOBA_BLOCK - S
    kp = jnp.pad(k, ((0, 0), (0, 0), (0, pad), (0, 0)))
    vp = jnp.pad(v, ((0, 0), (0, 0), (0, pad), (0, 0)))
    kb = kp.reshape(B, H, nb, MOBA_BLOCK, Dh)
    vb = vp.reshape(B, H, nb, MOBA_BLOCK, Dh)
    k_mean = jnp.mean(kb, axis=3)
    n_sel = min(MOBA_TOPK, nb)
    scale = Dh ** -0.5
    blk_ids = jnp.arange(nb)
    gather = jax.vmap(jax.vmap(lambda blocks, ids: blocks[ids]))

    def one_block(c):
        q0 = c * Q_BLOCK
        own = q0 // MOBA_BLOCK
        qc = lax.dynamic_slice_in_dim(q, q0, Q_BLOCK, axis=2)
        qpos = q0 + jnp.arange(Q_BLOCK)
        gate = jnp.einsum('bhqd,bhnd->bhqn', qc, k_mean)
        gate = jnp.where(blk_ids < own, gate, -jnp.inf)
        _, sel = lax.top_k(gate, n_sel)
        valid = sel < own
        k_sel = gather(kb, sel)
        v_sel = gather(vb, sel)
        s_sel = jnp.einsum('bhqd,bhqnkd->bhqnk', qc, k_sel) * scale
        s_sel = jnp.where(valid[..., None], s_sel, -jnp.inf).reshape(B, H, Q_BLOCK, n_sel * MOBA_BLOCK)
        k_own = lax.dynamic_slice_in_dim(kp, own * MOBA_BLOCK, MOBA_BLOCK, axis=2)
        v_own = lax.dynamic_slice_in_dim(vp, own * MOBA_BLOCK, MOBA_BLOCK, axis=2)
        kpos = own * MOBA_BLOCK + jnp.arange(MOBA_BLOCK)
        s_own = jnp.einsum('bhqd,bhkd->bhqk', qc, k_own) * scale
        s_own = jnp.where(kpos[None, :] <= qpos[:, None], s_own, -jnp.inf)
        p = jax.nn.softmax(jnp.concatenate([s_sel, s_own], axis=-1).astype(jnp.float32), axis=-1)
        p_sel = p[..., :n_sel * MOBA_BLOCK].reshape(B, H, Q_BLOCK, n_sel, MOBA_BLOCK)
        p_own = p[..., n_sel * MOBA_BLOCK:]
        return (jnp.einsum('bhqnk,bhqnkd->bhqd', p_sel, v_sel)
                + jnp.einsum('bhqk,bhkd->bhqd', p_own, v_own))

    out = lax.map(one_block, jnp.arange(S // Q_BLOCK))
    return jnp.transpose(out, (1, 2, 0, 3, 4)).reshape(B, H, S, Dh)


def mlstm_chunkwise(q, k, v, ig, lf):
    B, H, S, dk = q.shape
    dv = v.shape[-1]
    L = MLSTM_CHUNK
    nc = S // L

    def chunks(a):
        return jnp.moveaxis(a.reshape((B, H, nc, L) + a.shape[3:]), 2, 0)

    causal = jnp.tril(jnp.ones((L, L), dtype=bool))

    def step(carry, xs):
        C, n, m = carry
        qc, kc, vc, ic, fc = xs
        b = jnp.cumsum(fc, axis=-1)
        d = b[..., :, None] - b[..., None, :] + ic[..., None, :]
        d = jnp.where(causal, d, -jnp.inf)
        m_inter = b + m[..., None]
        m_t = jnp.maximum(m_inter, jnp.max(d, axis=-1))
        s = jnp.einsum('bhtd,bhsd->bhts', qc, kc) * jnp.exp(d - m_t[..., None])
        decay = jnp.exp(m_inter - m_t)
        num = (jnp.einsum('bhts,bhsv->bhtv', s, vc)
               + decay[..., None] * jnp.einsum('bhtd,bhdv->bhtv', qc, C))
        den = jnp.sum(s, axis=-1) + decay * jnp.einsum('bhtd,bhd->bht', qc, n)
        h = num / jnp.maximum(jnp.abs(den), jnp.exp(-m_t))[..., None]
        b_last = b[..., -1]
        g = b_last[..., None] - b + ic
        m_new = jnp.maximum(b_last + m, jnp.max(g, axis=-1))
        carry_decay = jnp.exp(b_last + m - m_new)
        wg = jnp.exp(g - m_new[..., None])
        C_new = carry_decay[..., None, None] * C + jnp.einsum('bhs,bhsd,bhsv->bhdv', wg, kc, vc)
        n_new = carry_decay[..., None] * n + jnp.einsum('bhs,bhsd->bhd', wg, kc)
        return (C_new, n_new, m_new), h

    init = (jnp.zeros((B, H, dk, dv), jnp.float32),
            jnp.zeros((B, H, dk), jnp.float32),
            jnp.zeros((B, H), jnp.float32))
    _, h = lax.scan(step, init, (chunks(q), chunks(k), chunks(v), chunks(ig), chunks(lf)))
    return jnp.moveaxis(h, 0, 2).reshape(B, H, S, dv)


def hybrid_layer(x, positions, g_mix_pre, w_in, b_igate, b_fgate, g_mlstm_out, w_out,
                 g_mix_post, g_mlp_pre, w_up, w_down, g_mlp_post):
    B, S, _ = x.shape
    hdn = rms_norm(x, g_mix_pre)
    proj = hdn @ w_in
    aq, ak, av, mq, mk, mv, mo, mi, mf = jnp.split(proj, [int(c) for c in np.cumsum(IN_SPLITS)[:-1]], axis=-1)

    def to_heads(t, nh):
        return t.reshape(B, S, nh, -1).transpose(0, 2, 1, 3).astype(jnp.float32)

    q_a = partial_rope(to_heads(aq, ATT_HEADS), positions)
    k_a = partial_rope(to_heads(ak, ATT_HEADS), positions)
    v_a = to_heads(av, ATT_HEADS)
    att = moba_attention(q_a, k_a, v_a)
    att = att.transpose(0, 2, 1, 3).reshape(B, S, ATT_W)

    q_m = to_heads(mq, MLSTM_HEADS)
    k_m = to_heads(mk, MLSTM_HEADS) * (MLSTM_QK_DIM ** -0.5)
    v_m = to_heads(mv, MLSTM_HEADS)
    ig = (mi.astype(jnp.float32) + b_igate.astype(jnp.float32)).transpose(0, 2, 1)
    lf = jax.nn.log_sigmoid(mf.astype(jnp.float32) + b_fgate.astype(jnp.float32)).transpose(0, 2, 1)
    hm = mlstm_chunkwise(q_m, k_m, v_m, ig, lf)
    hm = hm * lax.rsqrt(jnp.mean(hm * hm, axis=-1, keepdims=True) + EPS)
    hm = hm.transpose(0, 2, 1, 3).reshape(B, S, ML_V_W) * g_mlstm_out.astype(jnp.float32)
    hm = hm * jax.nn.sigmoid(mo.astype(jnp.float32))

    mixed = jnp.concatenate([att, hm], axis=-1).astype(x.dtype)
    x = x + rms_norm(mixed @ w_out, g_mix_post)

    u = rms_norm(x, g_mlp_pre) @ w_up
    u = jnp.square(jax.nn.relu(u))
    x = x + rms_norm(u @ w_down, g_mlp_post)
    return x


def setup_inputs(seed: int = 0) -> dict:
    key = jax.random.key(seed)
    ks = jax.random.split(key, 14)
    f32 = jnp.float32

    def gain(k, n):
        return 1.0 + 0.02 * jax.random.normal(k, (DEPTH, n), f32)

    x = jax.random.normal(ks[0], (BATCH, SEQ, D_MODEL), f32)
    positions = jnp.broadcast_to(jnp.arange(SEQ, dtype=jnp.int32), (BATCH, SEQ))
    g_mix_pre = gain(ks[1], D_MODEL)
    w_in = jax.random.normal(ks[2], (DEPTH, D_MODEL, IN_WIDTH), f32) * D_MODEL ** -0.5
    b_igate = 0.1 * jax.random.normal(ks[3], (DEPTH, MLSTM_HEADS), f32)
    b_fgate = (jnp.linspace(3.0, 6.0, MLSTM_HEADS, dtype=f32)[None, :]
               + 0.1 * jax.random.normal(ks[4], (DEPTH, MLSTM_HEADS), f32))
    g_mlstm_out = gain(ks[5], ML_V_W)
    w_out = jax.random.normal(ks[6], (DEPTH, MIX_WIDTH, D_MODEL), f32) * MIX_WIDTH ** -0.5
    g_mix_post = gain(ks[7], D_MODEL)
    g_mlp_pre = gain(ks[8], D_MODEL)
    w_up = jax.random.normal(ks[9], (DEPTH, D_MODEL, D_FF), f32) * D_MODEL ** -0.5
    w_down = jax.random.normal(ks[10], (DEPTH, D_FF, D_MODEL), f32) * D_FF ** -0.5
    g_mlp_post = gain(ks[11], D_MODEL)
    return {"x": x, "positions": positions, "g_mix_pre": g_mix_pre, "w_in": w_in,
            "b_igate": b_igate, "b_fgate": b_fgate, "g_mlstm_out": g_mlstm_out, "w_out": w_out,
            "g_mix_post": g_mix_post, "g_mlp_pre": g_mlp_pre, "w_up": w_up, "w_down": w_down,
            "g_mlp_post": g_mlp_post}


def reference(x, positions, g_mix_pre, w_in, b_igate, b_fgate, g_mlstm_out, w_out,
              g_mix_post, g_mlp_pre, w_up, w_down, g_mlp_post):
    for layer in range(DEPTH):
        x = hybrid_layer(x, positions, g_mix_pre[layer], w_in[layer], b_igate[layer], b_fgate[layer],
                         g_mlstm_out[layer], w_out[layer], g_mix_post[layer], g_mlp_pre[layer],
                         w_up[layer], w_down[layer], g_mlp_post[layer])
    return x
```

```python
import numpy as np
import ml_dtypes
from contextlib import ExitStack
import concourse.bass as bass
import concourse.mybir as mybir
from concourse.bass_utils import run_bass_kernel_spmd

F32 = mybir.dt.float32
BF16 = mybir.dt.bfloat16
I32 = mybir.dt.int32
AF = mybir.ActivationFunctionType
ALU = mybir.AluOpType
AX = mybir.AxisListType
NPBF = ml_dtypes.bfloat16

D = 1024
S = 8192
NB = 2
DEPTH = 2
NCORE = 8
G = 512
NG = 4
T = G * NG
INW = 3080
DFF = 4096
EPS = 1e-6
ROPE_THETA = 500000.0
NEG = -30000.0


def core_groups(j):
    return [j, 7 - j, 8 + j, 15 - j]


class Prog:
    NDMA = 24
    CH = 20000

    def __init__(self, nc, stack, same_engine_sync=True):
        self.nc = nc
        self.stack = stack
        self.ops = []
        self.same = same_engine_sync
        self.engs = {"pe": nc.tensor, "act": nc.scalar, "dve": nc.vector,
                     "pool": nc.gpsimd, "sp": nc.sync}

    def op(self, eng, meth, *args, r=(), w=(), **kw):
        self.ops.append(dict(eng=eng, fn=(meth, args, kw), r=tuple(r), w=tuple(w), dma=False))

    def dma(self, q, out, in_, r=(), w=(), **kw):
        self.ops.append(dict(eng=q, fn=("dma_start", (), dict(out=out, in_=in_, **kw)),
                             r=tuple(r), w=tuple(w), dma=True))

    def _run(self, o):
        meth, args, kw = o["fn"]
        return getattr(self.engs[o["eng"]], meth)(*args, **kw)

    def emit(self):
        nc = self.nc
        ops = self.ops
        n = len(ops)
        last_w = {}
        readers = {}
        deps = [None] * n
        for i, o in enumerate(ops):
            d = set()
            for k in o["r"]:
                if k in last_w:
                    d.add(last_w[k])
            for k in o["w"]:
                if k in last_w:
                    d.add(last_w[k])
                d.update(readers.get(k, ()))
            for k in o["r"]:
                readers.setdefault(k, []).append(i)
            for k in o["w"]:
                last_w[k] = i
                readers[k] = []
            d.discard(i)
            dd = set()
            for j in d:
                oj = ops[j]
                if not oj["dma"] and not o["dma"] and oj["eng"] == o["eng"]:
                    if o["eng"] == "pe" or not self.same:
                        continue
                dd.add(j)
            deps[i] = dd
        needs = [False] * n
        for i in range(n):
            for j in deps[i]:
                needs[j] = True
        tot = {}
        for i, o in enumerate(ops):
            if not o["dma"] and needs[i]:
                tot[o["eng"]] = tot.get(o["eng"], 0) + 1
        sems = {}
        for e, c in tot.items():
            sems[e] = [self.stack.enter_context(nc.semaphore(f"s_{e}_{k}"))
                       for k in range((c + self.CH - 1) // self.CH)]
        ndma = sum(1 for o in ops if o["dma"])
        nd = min(self.NDMA, max(1, ndma))
        dsems = [self.stack.enter_context(nc.semaphore(f"s_dma_{k}")) for k in range(nd)]
        cnt = {}
        target = [None] * n
        waited = {}
        dcount = 0
        dfinal = {}

        def do_wait(eng, sem, val):
            key = (eng, id(sem))
            if waited.get(key, 0) < val:
                self.engs[eng].wait_ge(sem, val)
                waited[key] = val

        for i, o in enumerate(ops):
            e = o["eng"]
            for j in sorted(deps[i]):
                sem, val = target[j]
                do_wait(e, sem, val)
            if o["dma"]:
                k = dcount % nd
                rnd = dcount // nd
                if rnd > 0:
                    do_wait(e, dsems[k], 16 * rnd)
                inst = self._run(o)
                inst.then_inc(dsems[k], 16)
                target[i] = (dsems[k], 16 * (rnd + 1))
                dfinal[k] = 16 * (rnd + 1)
                dcount += 1
            else:
                inst = self._run(o)
                if needs[i]:
                    c = cnt.get(e, 0)
                    sem = sems[e][c // self.CH]
                    val = c % self.CH + 1
                    inst.then_inc(sem, 1)
                    target[i] = (sem, val)
                    cnt[e] = c + 1
        for k, v in dfinal.items():
            do_wait("sp", dsems[k], v)
        for e, c in cnt.items():
            if c > 0:
                do_wait("sp", sems[e][(c - 1) // self.CH], (c - 1) % self.CH + 1)
        self.stats = dict(n_ops=n, n_dma=ndma, incs=dict(cnt))


class Ctx:
    def __init__(self):
        self.nc = bass.Bass("TRN2", target_bir_lowering=False)
        self.st = ExitStack()
        self.P = Prog(self.nc, self.st)
        self.uid = 0

    def din(self, name, shape, dt):
        return self.nc.dram_tensor(name, list(shape), dt, kind="ExternalInput").ap()

    def dout(self, name, shape, dt):
        return self.nc.dram_tensor(name, list(shape), dt, kind="ExternalOutput").ap()

    def sb(self, name, shape, dt):
        return self.st.enter_context(self.nc.sbuf_tensor(name, list(shape), dt))

    def psum(self, name, shape, dt=F32):
        return self.st.enter_context(self.nc.psum_tensor(name, list(shape), dt))

    def finish(self):
        self.P.emit()
        self.st.close()
        return self.nc


class Ring:
    def __init__(self, tiles, name):
        self.tiles = tiles
        self.name = name
        self.i = 0

    def next(self):
        k = self.i % len(self.tiles)
        self.i += 1
        return self.tiles[k], (self.name, k)


NCOLA = INW + 1024


def build_A(debug=False):
    c = Ctx()
    nc, P = c.nc, c.P
    xT = c.din("xT", [D, T], F32)
    pos = c.din("pos", [1, T], I32)
    wA = c.din("wA", [D, NCOLA], F32)
    gpre = c.din("gpre", [128, 8], F32)
    gbias = c.din("gbias", [4, 2], F32)
    fcs = c.din("fcs", [128, 2], F32)
    o_qT = c.dout("qT", [512, T], BF16)
    o_kT = c.dout("kT", [512, T], BF16)
    o_vT = c.dout("vT", [512, T], BF16)
    o_kmT = c.dout("kmT", [512, 2 * NG], BF16)
    o_mqT = c.dout("mqT", [256, T], BF16)
    o_mkT = c.dout("mkT", [256, T], BF16)
    o_mvT = c.dout("mvT", [512, T], BF16)
    o_sgT = c.dout("sgT", [512, T], F32)
    o_ig = c.dout("igT", [4, T], F32)
    o_lf = c.dout("lfT", [4, T], F32)
    o_b = c.dout("bT", [4, T], F32)

    wbf = c.sb("wbf", [128, 8, NCOLA], BF16)
    x_sb = [c.sb(f"x{i}", [128, 8, G], F32) for i in range(2)]
    sq = c.sb("sq", [128, 8, G], BF16)
    hT = c.sb("hT", [128, 8, G], BF16)
    rstd = c.sb("rstd", [128, G], F32)
    lnv = c.sb("lnv", [128, G], F32)
    ones_bf = c.sb("ones_bf", [128, 128], BF16)
    ones4 = c.sb("ones4", [4, G], F32)
    gpre_sb = c.sb("gpre_sb", [128, 8], F32)
    gb_sb = c.sb("gb_sb", [4, 2], F32)
    ngb_sb = c.sb("ngb_sb", [4, 2], F32)
    fcs_sb = c.sb("fcs_sb", [128, 2], F32)
    posi = c.sb("posi", [128, G], I32)
    posf = c.sb("posf", [128, G], F32)
    yy = c.sb("yy", [128, G], F32)
    nn = c.sb("nn", [128, G], F32)
    Ct = c.sb("Ct", [128, G], F32)
    St = c.sb("St", [128, G], F32)
    t1 = [c.sb(f"t1_{i}", [128, G], F32) for i in range(2)]
    t2 = [c.sb(f"t2_{i}", [128, G], F32) for i in range(2)]
    obf = Ring([c.sb(f"obf{i}", [128, G], BF16) for i in range(6)], "obf")
    o32 = Ring([c.sb(f"o32{i}", [128, G], F32) for i in range(3)], "o32")
    km = c.sb("km", [128, 4, 2 * NG], F32)
    km_bf = c.sb("km_bf", [128, 4, 2 * NG], BF16)
    g4 = [c.sb(f"g4_{i}", [4, G], F32) for i in range(5)]
    ps = Ring([c.psum(f"ps{i}", [128, G]) for i in range(8)], "ps")

    P.op("pool", "memset", ones_bf[:], 1.0, w=["ones_bf"])
    P.op("pool", "memset", ones4[:], 1.0, w=["ones4"])
    P.dma("sp", gpre_sb[:], gpre, w=["gpre"])
    P.dma("sp", gb_sb[:], gbias, w=["gb"])
    P.dma("sp", fcs_sb[:], fcs, w=["fcs"])
    P.op("dve", "tensor_scalar", ngb_sb[:], gb_sb[:], -1.0, None, ALU.mult, r=["gb"], w=["ngb"])
    for k in range(8):
        P.dma("pool", wbf[:, k, :], wA[k * 128:(k + 1) * 128, :], w=[("wbf", k)])
    xTv = xT.rearrange("(k p) t -> p k t", p=128)

    def load_x(g):
        P.dma("sp", x_sb[g % 2][:], xTv[:, :, g * G:(g + 1) * G], w=[("x", g % 2)])

    load_x(0)
    for g in range(NG):
        xs = x_sb[g % 2]
        xk = ("x", g % 2)
        cols = slice(g * G, (g + 1) * G)
        if g + 1 < NG:
            load_x(g + 1)
        P.dma("sp", posi[:], pos[0:1, cols].broadcast_to([128, G]), w=["posi"])
        P.op("dve", "tensor_copy", posf[:], posi[:], r=["posi"], w=["posf"])
        for which, tab in ((0, Ct), (1, St)):
            off = 0.25 if which == 0 else 0.0
            P.op("dve", "tensor_scalar", yy[:], posf[:], fcs_sb[:, which:which + 1], off, ALU.mult, ALU.add,
                 r=["posf", "fcs"], w=["yy"])
            P.op("dve", "tensor_scalar", nn[:], yy[:], 12582912.0, 12582912.0, ALU.add, ALU.subtract,
                 r=["yy"], w=["nn"])
            P.op("dve", "tensor_tensor", yy[:], yy[:], nn[:], ALU.subtract, r=["yy", "nn"], w=["yy"])
            P.op("act", "activation", tab[:], yy[:], AF.Sin, scale=6.28318, r=["yy"], w=[("tab", which)])
        for k in range(8):
            if k % 2 == 0:
                P.op("act", "activation", sq[:, k, :], xs[:, k, :], AF.Square, r=[xk], w=[("sq", k)])
            else:
                P.op("pool", "tensor_tensor", sq[:, k, :], xs[:, k, :], xs[:, k, :], ALU.mult, r=[xk], w=[("sq", k)])
        pt, pk = ps.next()
        for k in range(8):
            P.op("pe", "matmul", pt[:], ones_bf[:], sq[:, k, :], start=(k == 0), stop=(k == 7),
                 r=["ones_bf", ("sq", k)], w=[pk])
        P.op("act", "activation", lnv[:], pt[:], AF.Ln, scale=1.0 / D, bias=EPS, r=[pk], w=["lnv"])
        P.op("act", "activation", rstd[:], lnv[:], AF.Exp, scale=-0.5, r=["lnv"], w=["rstd"])
        for k in range(8):
            P.op("dve", "scalar_tensor_tensor", hT[:, k, :], xs[:, k, :], gpre_sb[:, k:k + 1], rstd[:],
                 ALU.mult, ALU.mult, r=[xk, "gpre", "rstd"], w=[("hT", k)])

        def proj(col0, m):
            pt, pk = ps.next()
            for k in range(8):
                P.op("pe", "matmul", pt[0:m, :], wbf[:, k, col0:col0 + m], hT[:, k, :],
                     start=(k == 0), stop=(k == 7), r=[("wbf", k), ("hT", k)], w=[pk])
            return pt, pk

        for qi, (base, pbase, dst) in enumerate(((0, INW, o_qT), (512, INW + 512, o_kT))):
            for ch in range(4):
                pa, pak = proj(base + 128 * ch, 128)
                pb, pbk = proj(pbase + 128 * ch, 128)
                ta, tb = t1[ch % 2], t2[ch % 2]
                P.op("dve", "tensor_tensor", ta[:], pa[:], Ct[:], ALU.mult, r=[pak, ("tab", 0)], w=[("t1", ch % 2)])
                P.op("dve", "tensor_tensor", tb[:], pb[:], St[:], ALU.mult, r=[pbk, ("tab", 1)], w=[("t2", ch % 2)])
                ot, ok = obf.next()
                P.op("pool", "tensor_tensor", ot[:], ta[:], tb[:], ALU.add,
                     r=[("t1", ch % 2), ("t2", ch % 2)], w=[ok])
                P.dma("sp", dst[128 * ch:128 * (ch + 1), cols], ot[:], r=[ok])
                if qi == 1:
                    P.op("dve", "reduce_sum", km[:, ch, 2 * g:2 * g + 2],
                         ot[:].rearrange("p (b t) -> p b t", b=2), AX.X, r=[ok], w=[("km", ch)])
        plain = [(1024 + 128 * i, o_vT, i, 1.0) for i in range(4)]
        plain += [(1536 + 128 * i, o_mqT, i, 1.0) for i in range(2)]
        plain += [(1792 + 128 * i, o_mkT, i, 0.125) for i in range(2)]
        plain += [(2048 + 128 * i, o_mvT, i, 1.0) for i in range(4)]
        for n_, (col0, dst, ch, scl) in enumerate(plain):
            pa, pak = proj(col0, 128)
            ot, ok = obf.next()
            if n_ % 2 == 0:
                P.op("act", "activation", ot[:], pa[:], AF.Copy, scale=scl, r=[pak], w=[ok])
            else:
                P.op("dve", "tensor_scalar", ot[:], pa[:], scl, None, ALU.mult, r=[pak], w=[ok])
            P.dma("sp", dst[128 * ch:128 * (ch + 1), cols], ot[:], r=[ok])
        for ch in range(4):
            pa, pak = proj(2560 + 128 * ch, 128)
            ot, ok = o32.next()
            P.op("act", "activation", ot[:], pa[:], AF.Sigmoid, r=[pak], w=[ok])
            P.dma("sp", o_sgT[128 * ch:128 * (ch + 1), cols], ot[:], r=[ok])
        pa, pak = proj(3072, 4)
        P.op("dve", "tensor_scalar", g4[0][:], pa[0:4, :], gb_sb[:, 0:1], None, ALU.add, r=[pak, "gb"], w=[("g4", 0)])
        P.dma("sp", o_ig[:, cols], g4[0][:], r=[("g4", 0)])
        pa, pak = proj(3076, 4)
        P.op("act", "activation", g4[1][:], pa[0:4, :], AF.Exp, scale=-1.0, bias=ngb_sb[:, 1:2],
             r=[pak, "ngb"], w=[("g4", 1)])
        P.op("act", "activation", g4[2][:], g4[1][:], AF.Ln, bias=1.0, r=[("g4", 1)], w=[("g4", 2)])
        P.op("dve", "tensor_scalar", g4[3][:], g4[2][:], -1.0, None, ALU.mult, r=[("g4", 2)], w=[("g4", 3)])
        P.dma("sp", o_lf[:, cols], g4[3][:], r=[("g4", 3)])
        P.op("dve", "tensor_tensor_scan", g4[4][:], ones4[:], g4[3][:], 0.0, ALU.mult, ALU.add,
             r=["ones4", ("g4", 3)], w=[("g4", 4)])
        P.dma("sp", o_b[:, cols], g4[4][:], r=[("g4", 4)])
    for ch in range(4):
        P.op("dve", "tensor_scalar", km_bf[:, ch, :], km[:, ch, :], 1.0 / 256, None, ALU.mult,
             r=[("km", ch)], w=[("kmb", ch)])
        P.dma("sp", o_kmT[128 * ch:128 * (ch + 1), :], km_bf[:, ch, :], r=[("kmb", ch)])
    return c.finish()


def rope_consts():
    inv = ROPE_THETA ** (-np.arange(0, 16, 2, dtype=np.float32) / 16.0)
    f = (inv.astype(np.float64) / (2 * np.pi)).astype(np.float32)
    fc = np.zeros((128, 2), np.float32)
    for hh in range(2):
        for d in range(16):
            p = 64 * hh + d
            fc[p, 0] = f[d % 8]
            fc[p, 1] = -f[d % 8] if d < 8 else f[d % 8]
    return fc


def rope_perm():
    perm = np.arange(512)
    for h in range(8):
        for d in range(16):
            perm[64 * h + d] = 64 * h + (d + 8 if d < 8 else d - 8)
    return perm


def token_index(j):
    return np.concatenate([np.arange(g * G, (g + 1) * G) for g in core_groups(j)])


NT_SLOT = [16, 32, 48, 64]
NT_OFF = [0, 16, 48, 96]
NT_ALL = 160
NSEG = 16


def build_B(att=True, mlstm=True):
    c = Ctx()
    nc, P = c.nc, c.P
    qh = c.din("qh", [8, 64, T], BF16)
    kh = c.din("kh", [8, 64, NT_ALL * 128], BF16)
    eh = c.din("eh", [32, NT_ALL * 128], BF16)
    vh = c.din("vh", [8, 128, NT_ALL, 65], BF16)
    kmh = c.din("kmh", [8, 64, 32], BF16)
    gmask = c.din("gmask", [128, 16, 32], F32)
    mask2 = c.din("mask2", [128, 16, 32], F32)
    ident = c.din("ident", [128, 128], BF16)
    tri = c.din("tri", [128, 128], BF16)
    trif = c.din("trif", [128, 128], F32)
    mqh = c.din("mqh", [4, 64, T], BF16)
    mkh = c.din("mkh", [4, 64, T], BF16)
    mvo = c.din("mvo", [128, 16, 4, 129], BF16)
    sg = c.din("sg", [512, T], F32)
    brow = c.din("brow", [4, T], F32)
    igo = c.din("igo", [128, 16, 4], F32)
    bo = c.din("bo", [128, 16, 4], F32)
    mka = c.din("mka", [NSEG, 128, 4, 256], BF16)
    mva = c.din("mva", [NSEG, 128, 4, 4, 129], BF16)
    iga = c.din("iga", [128, 64, 4], F32)
    ba = c.din("ba", [128, 64, 4], F32)
    brep = c.din("brep", [128, NSEG, 4], F32)
    mcol = c.din("mcol", [64, 4, NSEG], F32)
    gml = c.din("gml", [128, 4], F32)
    o_att = c.dout("attT", [8, 64, T], BF16)
    o_mm = c.dout("mmT", [512, T], BF16)

    psb = [c.psum(f"ps{i}", [128, G]) for i in range(8)]
    rA = Ring(psb[0:3], "psA")
    rB = Ring(psb[3:5], "psB")
    rC = Ring(psb[5:8], "psC")
    ones_bf = c.sb("ones_bf", [128, 128], BF16)
    ones_f = c.sb("ones_f", [128, 128], F32)
    P.op("pool", "memset", ones_bf[:], 1.0, w=["ones_bf"])
    P.op("pool", "memset", ones_f[:], 1.0, w=["ones_f"])
    ident_sb = c.sb("ident_sb", [128, 128], BF16)
    tri_sb = c.sb("tri_sb", [128, 128], BF16)
    trif_sb = c.sb("trif_sb", [128, 128], F32)
    P.dma("sp", ident_sb[:], ident, w=["ident"])
    P.dma("sp", tri_sb[:], tri, w=["tri"])
    P.dma("sp", trif_sb[:], trif, w=["trif"])

    if mlstm:
        iga_sb = c.sb("iga_sb", [128, 64, 4], F32)
        ba_sb = c.sb("ba_sb", [128, 64, 4], F32)
        brep_sb = c.sb("brep_sb", [128, NSEG, 4], F32)
        warg = c.sb("warg", [128, 64, 4], F32)
        wts = c.sb("wts", [128, 64, 4], F32)
        eB = c.sb("eB", [128, NSEG, 4], F32)
        mcol_sb = c.sb("mcol_sb", [64, 4, NSEG], F32)
        gml_sb = c.sb("gml_sb", [128, 4], F32)
        P.dma("sp", iga_sb[:], iga, w=["iga"])
        P.dma("sp", ba_sb[:], ba, w=["ba"])
        P.dma("sp", brep_sb[:], brep, w=["brep"])
        P.dma("sp", mcol_sb[:], mcol, w=["mcol"])
        P.dma("sp", gml_sb[:], gml, w=["gml"])
        P.op("dve", "tensor_tensor", warg[:], iga_sb[:], ba_sb[:], ALU.subtract, r=["iga", "ba"], w=["warg"])
        for s_ in range(NSEG):
            P.op("dve", "tensor_tensor", warg[:, 4 * s_:4 * s_ + 4, :], warg[:, 4 * s_:4 * s_ + 4, :],
                 brep_sb[:, s_:s_ + 1, :].broadcast_to([128, 4, 4]), ALU.add, r=["warg", "brep"], w=["warg"])
        P.op("act", "activation", wts[:], warg[:], AF.Exp, r=["warg"], w=["wts"])
        P.op("act", "activation", eB[:], brep_sb[:], AF.Exp, r=["brep"], w=["eB"])
        cloc = c.sb("cloc", [64, NSEG, 4, 129], F32)
        mk_st = [c.sb(f"mk_st{i}", [128, 4, 256], BF16) for i in range(2)]
        mv_st = [c.sb(f"mv_st{i}", [128, 4, 4, 129], BF16) for i in range(2)]
        kw = Ring([c.sb(f"kw{i}", [128, 64], BF16) for i in range(4)], "kw")
        for s_ in range(NSEG):
            bi = s_ % 2
            P.dma("sp", mk_st[bi][:], mka[s_], w=[("mk_st", bi)])
            P.dma("sp", mv_st[bi][:], mva[s_], w=[("mv_st", bi)])
            for h in range(4):
                pt, pk = rC.next()
                for a in range(4):
                    kt, kk = kw.next()
                    eng = "dve" if (a % 2 == 0) else "pool"
                    P.op(eng, "tensor_scalar", kt[:], mk_st[bi][:, a, 64 * h:64 * h + 64],
                         wts[:, 4 * s_ + a, h:h + 1], None, ALU.mult, r=[("mk_st", bi), "wts"], w=[kk])
                    P.op("pe", "matmul", pt[0:64, 0:129], kt[:], mv_st[bi][:, a, h, :],
                         start=(a == 0), stop=(a == 3), r=[kk, ("mv_st", bi)], w=[pk])
                P.op("act", "activation", cloc[:, s_, h, :], pt[0:64, 0:129], AF.Copy, r=[pk], w=[("cloc", s_, h)])
        eBm = c.sb("eBm", [64, 4, NSEG, 4], F32)
        for l in range(4):
            for h in range(4):
                P.op("dve", "scalar_tensor_tensor", eBm[:, l, :, h], eB[0:64, :, h], -1.0, mcol_sb[:, l, :],
                     ALU.add, ALU.mult, r=["eB", "mcol"], w=[("eBm", l, h)])
                P.op("dve", "tensor_scalar", eBm[:, l, :, h], eBm[:, l, :, h], 1.0, None, ALU.add,
                     r=[("eBm", l, h)], w=[("eBm", l, h)])
        st_ = [[c.sb(f"st{l}_{h}", [64, 129], F32) for h in range(4)] for l in range(4)]
        tmpS = Ring([c.sb(f"tmpS{i}", [64, 129], F32) for i in range(2)], "tmpS")
        cin_bf = [[c.sb(f"cin{l}_{h}", [64, 128], BF16) for h in range(4)] for l in range(4)]
        nin_bf = [[c.sb(f"nin{l}_{h}", [64, 128], BF16) for h in range(4)] for l in range(4)]
        for l in range(4):
            for h in range(4):
                S_ = st_[l][h]
                sk = ("st", l, h)
                P.op("pool", "memset", S_[:], 0.0, w=[sk])
                for s_ in range(NSEG - 1):
                    tt, tk = tmpS.next()
                    P.op("dve", "tensor_scalar", tt[:], S_[:], eBm[:, l, s_, h:h + 1], None, ALU.mult,
                         r=[sk, ("eBm", l, h)], w=[tk])
                    P.op("dve", "scalar_tensor_tensor", S_[:], cloc[:, s_, h, :], mcol_sb[:, l, s_:s_ + 1], tt[:],
                         ALU.mult, ALU.add, r=[("cloc", s_, h), "mcol", tk], w=[sk])
                P.op("act", "activation", cin_bf[l][h][:], S_[:, 0:128], AF.Copy, r=[sk], w=[("cin", l, h)])
                P.op("dve", "tensor_copy", nin_bf[l][h][:], S_[:, 128:129].broadcast_to([64, 128]),
                     r=[sk], w=[("nin", l, h)])
        igo_sb = c.sb("igo_sb", [128, 16, 4], F32)
        bo_sb = c.sb("bo_sb", [128, 16, 4], F32)
        negc = c.sb("negc", [128, 16, 4], F32)
        P.dma("sp", igo_sb[:], igo, w=["igo"])
        P.dma("sp", bo_sb[:], bo, w=["bo"])
        P.op("dve", "tensor_tensor", negc[:], igo_sb[:], bo_sb[:], ALU.subtract, r=["igo", "bo"], w=["negc"])
        mvo_sb = c.sb("mvo_sb", [128, 16, 4, 129], BF16)
        P.dma("sp", mvo_sb[:], mvo, w=["mvo"])
        mq_t = Ring([c.sb(f"mq_t{i}", [64, G], BF16) for i in range(2)], "mq_t")
        mk_t = Ring([c.sb(f"mk_t{i}", [64, G], BF16) for i in range(2)], "mk_t")
        sg_t = Ring([c.sb(f"sg_t{i}", [128, G], F32) for i in range(2)], "sg_t")
        br_t = Ring([c.sb(f"br_t{i}", [1, G], F32) for i in range(2)], "br_t")
        EB = c.sb("EB", [128, G], F32)
        Et = Ring([c.sb(f"Et{i}", [128, G], F32) for i in range(2)], "Et")
        Pm = Ring([c.sb(f"Pm{i}", [128, G], BF16) for i in range(2)], "Pm")
        QE = c.sb("QE", [64, G], BF16)
        dn = c.sb("dn", [128, G], F32)
        rdn = c.sb("rdn", [128, G], F32)
        hh = c.sb("hh", [128, G], F32)
        hsq = c.sb("hsq", [128, G], BF16)
        lnh = c.sb("lnh", [128, G], F32)
        rsh = c.sb("rsh", [128, G], F32)
        hn = c.sb("hn", [128, G], F32)
        mo_t = Ring([c.sb(f"mo_t{i}", [128, G], BF16) for i in range(2)], "mo_t")
        for l in range(4):
            cols = slice(l * G, (l + 1) * G)
            for h in range(4):
                mq, mqk = mq_t.next()
                mk, mkk = mk_t.next()
                sgt, sgk = sg_t.next()
                brt, brk = br_t.next()
                P.dma("sp", mq[:], mqh[h, :, cols], w=[mqk])
                P.dma("sp", mk[:], mkh[h, :, cols], w=[mkk])
                P.dma("sp", sgt[:], sg[128 * h:128 * h + 128, cols], w=[sgk])
                P.dma("sp", brt[:], brow[h:h + 1, cols], w=[brk])
                bb, bbk = rA.next()
                P.op("pe", "matmul", bb[:], ones_f[0:1, :], brt[:], start=True, stop=True, r=["ones_f", brk], w=[bbk])
                P.op("act", "activation", EB[:], bb[:], AF.Exp, r=[bbk], w=["EB"])
                num, numk = rB.next()
                den, denk = rB.next()
                for a in range(4):
                    n0 = 128 * a
                    et, ek = Et.next()
                    P.op("act", "activation", et[:, n0:G], bb[:, n0:G], AF.Exp, bias=negc[:, 4 * l + a, h:h + 1],
                         r=[bbk, "negc"], w=[ek])
                    P.op("pool", "tensor_tensor", et[:, n0:n0 + 128], et[:, n0:n0 + 128], trif_sb[:], ALU.mult,
                         r=[ek, "trif"], w=[ek])
                    gm, gmk = rC.next()
                    P.op("pe", "matmul", gm[:, n0:G], mk[:, n0:n0 + 128], mq[:, n0:G], start=True, stop=True,
                         r=[mkk, mqk], w=[gmk])
                    pm, pmk = Pm.next()
                    P.op("dve", "tensor_tensor", pm[:, n0:G], gm[:, n0:G], et[:, n0:G], ALU.mult, r=[gmk, ek], w=[pmk])
                    P.op("pe", "matmul", num[:, n0:G], mvo_sb[:, 4 * l + a, h, 0:128], pm[:, n0:G],
                         start=(a == 0), stop=False, r=["mvo", pmk], w=[numk])
                    P.op("pe", "matmul", den[:, n0:G], ones_bf[:], pm[:, n0:G],
                         start=(a == 0), stop=False, r=["ones_bf", pmk], w=[denk])
                P.op("pool", "tensor_tensor", QE[:], mq[:], EB[0:64, :], ALU.mult, r=[mqk, "EB"], w=["QE"])
                P.op("pe", "matmul", num[:], cin_bf[l][h][:], QE[:], start=False, stop=True,
                     r=[("cin", l, h), "QE"], w=[numk])
                P.op("pe", "matmul", den[:], nin_bf[l][h][:], QE[:], start=False, stop=True,
                     r=[("nin", l, h), "QE"], w=[denk])
                P.op("act", "activation", dn[:], den[:], AF.Abs, r=[denk], w=["dn"])
                P.op("dve", "tensor_scalar", dn[:], dn[:], 1.0, None, ALU.max, r=["dn"], w=["dn"])
                P.op("dve", "reciprocal", rdn[:], dn[:], r=["dn"], w=["rdn"])
                P.op("dve", "tensor_tensor", hh[:], num[:], rdn[:], ALU.mult, r=[numk, "rdn"], w=["hh"])
                P.op("pool", "tensor_tensor", hsq[:], hh[:], hh[:], ALU.mult, r=["hh"], w=["hsq"])
                ss, ssk = rA.next()
                P.op("pe", "matmul", ss[:], ones_bf[:], hsq[:], start=True, stop=True, r=["ones_bf", "hsq"], w=[ssk])
                P.op("act", "activation", lnh[:], ss[:], AF.Ln, scale=1.0 / 128, bias=EPS, r=[ssk], w=["lnh"])
                P.op("act", "activation", rsh[:], lnh[:], AF.Exp, scale=-0.5, r=["lnh"], w=["rsh"])
                P.op("dve", "tensor_tensor", hn[:], hh[:], rsh[:], ALU.mult, r=["hh", "rsh"], w=["hn"])
                mo, mok = mo_t.next()
                P.op("dve", "scalar_tensor_tensor", mo[:], hn[:], gml_sb[:, h:h + 1], sgt[:], ALU.mult, ALU.mult,
                     r=["hn", "gml", sgk], w=[mok])
                P.dma("sp", o_mm[128 * h:128 * h + 128, cols], mo[:], r=[mok])

    if att:
        gmask_sb = c.sb("gmask_sb", [128, 16, 32], F32)
        mask2_sb = c.sb("mask2_sb", [128, 16, 32], F32)
        P.dma("sp", gmask_sb[:], gmask, w=["gmask"])
        P.dma("sp", mask2_sb[:], mask2, w=["mask2"])
        kaug = [c.sb(f"kaug{i}", [96, 64 * 128], BF16) for i in range(2)]
        vaug = [c.sb(f"vaug{i}", [128, 64, 65], BF16) for i in range(2)]
        qaug = [c.sb(f"qaug{i}", [96, G], BF16) for i in range(2)]
        km_sb = c.sb("km_sb", [64, 8, 32], BF16)
        P.dma("sp", km_sb[:], kmh.rearrange("h p n -> p h n"), w=["km"])
        gm_s = c.sb("gm_s", [128, 32], F32)
        m8 = c.sb("m8", [128, 8], F32)
        mbf = c.sb("mbf", [128, 32], F32)
        mbq = c.sb("mbq", [128, 96], BF16)
        P.op("pool", "memset", mbq[:], 0.0, w=["mbq"])
        pT = Ring([c.sb(f"pT{i}", [128, G], BF16) for i in range(3)], "pT")
        dn_a = c.sb("dn_a", [128, G], F32)
        bc_a = c.sb("bc_a", [64, G], F32)
        ao = Ring([c.sb(f"ao{i}", [64, G], BF16) for i in range(2)], "ao")
        it = 0
        for h in range(8):
            for l in range(4):
                bi = it % 2
                it += 1
                nt = NT_SLOT[l]
                k0 = NT_OFF[l] * 128
                cols = slice(l * G, (l + 1) * G)
                ka, va, qa = kaug[bi], vaug[bi], qaug[bi]
                kk_, vk_, qk_ = ("kaug", bi), ("vaug", bi), ("qaug", bi)
                P.dma("sp", ka[0:64, 0:nt * 128], kh[h, :, k0:k0 + nt * 128], w=[kk_])
                P.dma("sp", ka[64:96, 0:nt * 128], eh[:, k0:k0 + nt * 128], w=[kk_])
                P.dma("sp", va[:, 0:nt, :], vh[h, :, NT_OFF[l]:NT_OFF[l] + nt, :], w=[vk_])
                P.dma("sp", qa[0:64, :], qh[h, :, cols], w=[qk_])
                for qt in range(4):
                    gp, gpk = rC.next()
                    P.op("pe", "matmul", gp[:, 0:32], qa[0:64, 128 * qt:128 * qt + 128], km_sb[:, h, :],
                         start=True, stop=True, r=[qk_, "km"], w=[gpk])
                    P.op("dve", "tensor_tensor", gm_s[:], gp[:, 0:32], gmask_sb[:, 4 * l + qt, :], ALU.add,
                         r=[gpk, "gmask"], w=["gm_s"])
                    P.op("dve", "max", m8[:], gm_s[:], r=["gm_s"], w=["m8"])
                    P.op("dve", "tensor_tensor", mbf[:], gm_s[:], m8[:, 3:4].broadcast_to([128, 32]), ALU.is_lt,
                         r=["gm_s", "m8"], w=["mbf"])
                    P.op("dve", "scalar_tensor_tensor", mbq[:, 64:96], mbf[:], NEG, mask2_sb[:, 4 * l + qt, :],
                         ALU.mult, ALU.add, r=["mbf", "mask2"], w=["mbq"])
                    tp, tpk = rC.next()
                    P.op("pe", "matmul", tp[0:96, 0:128], mbq[:], ident_sb[:], start=True, stop=True,
                         r=["mbq", "ident"], w=[tpk])
                    P.op("act", "activation", qa[64:96, 128 * qt:128 * qt + 128], tp[64:96, 0:128], AF.Copy,
                         r=[tpk], w=[qk_])
                acc, acck = rB.next()
                for kt in range(nt):
                    n0 = 128 * kt if kt < 4 else 0
                    sp_, spk = rA.next()
                    P.op("pe", "matmul", sp_[:, n0:G], ka[:, 128 * kt:128 * kt + 128], qa[:, n0:G],
                         start=True, stop=True, r=[kk_, qk_], w=[spk])
                    pt, ptk = pT.next()
                    P.op("act", "activation", pt[:, n0:G], sp_[:, n0:G], AF.Exp, scale=0.125, r=[spk], w=[ptk])
                    if kt < 4:
                        P.op("pool", "tensor_tensor", pt[:, n0:n0 + 128], pt[:, n0:n0 + 128], tri_sb[:], ALU.mult,
                             r=[ptk, "tri"], w=[ptk])
                    P.op("pe", "matmul", acc[0:65, n0:G], va[:, kt, :], pt[:, n0:G],
                         start=(kt == 0), stop=(kt == nt - 1), r=[vk_, ptk], w=[acck])
                P.op("act", "activation", dn_a[64:65, :], acc[64:65, :], AF.Copy, r=[acck], w=["dn_a"])
                P.op("dve", "reciprocal", dn_a[64:65, :], dn_a[64:65, :], r=["dn_a"], w=["dn_a"])
                bcp, bcpk = rC.next()
                P.op("pe", "matmul", bcp[0:64, :], ones_f[64:65, 0:64], dn_a[64:65, :], start=True, stop=True,
                     r=["ones_f", "dn_a"], w=[bcpk])
                P.op("act", "activation", bc_a[:], bcp[0:64, :], AF.Copy, r=[bcpk], w=["bc_a"])
                at, atk = ao.next()
                P.op("dve", "tensor_tensor", at[:], acc[0:64, :], bc_a[:], ALU.mult, r=[acck, "bc_a"], w=[atk])
                P.dma("sp", o_att[h, :, cols], at[:], r=[atk])
    return c.finish()


def seq_assemble(outs, name, b):
    F_ = outs[4 * b][name].shape[0]
    full = np.zeros((F_, S), dtype=outs[4 * b][name].dtype)
    for j in range(4):
        a = np.asarray(outs[4 * b + j][name])
        for l, gq in enumerate(core_groups(j)):
            full[:, gq * G:(gq + 1) * G] = a[:, l * G:(l + 1) * G]
    return full


def key_tiles(j, l):
    gq = core_groups(j)[l]
    tl = list(range(4 * gq, 4 * gq + 4)) + list(range(0, 4 * gq))
    return tl + [-1] * (NT_SLOT[l] - len(tl))


def b_consts():
    ident = np.eye(128, dtype=np.float32)
    tri = (np.arange(128)[:, None] <= np.arange(128)[None, :]).astype(np.float32)
    return dict(ident=ident.astype(NPBF), tri=tri.astype(NPBF), trif=tri)


def b_inputs(outsA, g_mlstm_out):
    consts = b_consts()
    gml = np.ascontiguousarray(g_mlstm_out.reshape(4, 128).T).astype(np.float32)
    in_maps = []
    for b in range(NB):
        kT = seq_assemble(outsA, "kT", b)
        vT = seq_assemble(outsA, "vT", b)
        mkT = seq_assemble(outsA, "mkT", b)
        mvT = seq_assemble(outsA, "mvT", b)
        igT = seq_assemble(outsA, "igT", b)
        bT = seq_assemble(outsA, "bT", b)
        kmT = np.zeros((512, 32), dtype=NPBF)
        for j in range(4):
            a = np.asarray(outsA[4 * b + j]["kmT"])
            for l, gq in enumerate(core_groups(j)):
                kmT[:, 2 * gq:2 * gq + 2] = a[:, 2 * l:2 * l + 2]
        v_tok = np.ascontiguousarray(vT.T)
        mk_tok = np.ascontiguousarray(mkT.T)
        mv_tok = np.ascontiguousarray(mvT.T)
        mka = np.ascontiguousarray(mk_tok.reshape(NSEG, 4, 128, 256).transpose(0, 2, 1, 3))
        mva = np.ones((NSEG, 128, 4, 4, 129), dtype=NPBF)
        mva[..., 0:128] = mv_tok.reshape(NSEG, 4, 128, 4, 128).transpose(0, 2, 1, 3, 4)
        iga = np.ascontiguousarray(igT.T.reshape(64, 128, 4).transpose(1, 0, 2))
        ba = np.ascontiguousarray(bT.T.reshape(64, 128, 4).transpose(1, 0, 2))
        blast = bT[:, G - 1::G].T
        brep = np.ascontiguousarray(np.broadcast_to(blast[None], (128, NSEG, 4)))
        kmh = np.ascontiguousarray(kmT.reshape(8, 64, 32))
        for j in range(4):
            oa = outsA[4 * b + j]
            gs = core_groups(j)
            kh = np.zeros((8, 64, NT_ALL * 128), dtype=NPBF)
            eh = np.zeros((32, NT_ALL * 128), dtype=NPBF)
            vh = np.zeros((8, 128, NT_ALL, 65), dtype=NPBF)
            vh[..., 64] = 1
            gmask = np.zeros((16, 32), np.float32)
            mask2 = np.zeros((16, 32), np.float32)
            mcol = np.zeros((4, NSEG), np.float32)
            for l in range(4):
                for pos_, tl in enumerate(key_tiles(j, l)):
                    p0 = (NT_OFF[l] + pos_) * 128
                    if tl < 0:
                        eh[31, p0:p0 + 128] = 1
                        continue
                    kh[:, :, p0:p0 + 128] = kT[:, tl * 128:(tl + 1) * 128].reshape(8, 64, 128)
                    eh[tl // 2, p0:p0 + 128] = 1
                    vh[:, :, NT_OFF[l] + pos_, 0:64] = v_tok[tl * 128:(tl + 1) * 128].reshape(128, 8, 64).transpose(1, 0, 2)
                for qt in range(4):
                    own = 2 * gs[l] + qt // 2
                    gmask[4 * l + qt, own] = 1e30
                    gmask[4 * l + qt, own + 1:] = -1e30
                    mask2[4 * l + qt, own + 1:] = NEG
                mcol[l, :gs[l]] = 1.0
            own_tok = lambda a_: np.ascontiguousarray(np.asarray(a_).T)
            mvo = np.ones((128, 16, 4, 129), dtype=NPBF)
            mvo[..., 0:128] = own_tok(oa["mvT"]).reshape(16, 128, 4, 128).transpose(1, 0, 2, 3)
            igo = np.ascontiguousarray(own_tok(oa["igT"]).reshape(16, 128, 4).transpose(1, 0, 2))
            bo = np.ascontiguousarray(own_tok(oa["bT"]).reshape(16, 128, 4).transpose(1, 0, 2))
            in_maps.append(dict(
                qh=np.ascontiguousarray(np.asarray(oa["qT"]).reshape(8, 64, T)), kh=kh, eh=eh, vh=vh, kmh=kmh,
                gmask=np.ascontiguousarray(np.broadcast_to(gmask[None], (128, 16, 32))),
                mask2=np.ascontiguousarray(np.broadcast_to(mask2[None], (128, 16, 32))),
                mqh=np.ascontiguousarray(np.asarray(oa["mqT"]).reshape(4, 64, T)),
                mkh=np.ascontiguousarray(np.asarray(oa["mkT"]).reshape(4, 64, T)),
                mvo=mvo, sg=np.asarray(oa["sgT"]), brow=np.asarray(oa["bT"]), igo=igo, bo=bo,
                mka=mka, mva=mva, iga=iga, ba=ba, brep=brep,
                mcol=np.ascontiguousarray(np.broadcast_to(mcol[None], (64, 4, NSEG))), gml=gml, **consts))
    return in_maps


def build_O():
    c = Ctx()
    nc, P = c.nc, c.P
    mixT = c.din("mixT", [D, T], BF16)
    w_out = c.din("w_out", [D, D], F32)
    xT = c.din("xT", [D, T], F32)
    gpost = c.din("gpost", [128, 8], F32)
    o_x = c.dout("x1T", [D, T], F32)
    wo = c.sb("wo", [128, 8, D], BF16)
    g_sb = c.sb("g_sb", [128, 8], F32)
    ones_bf = c.sb("ones_bf", [128, 128], BF16)
    P.op("pool", "memset", ones_bf[:], 1.0, w=["ones_bf"])
    P.dma("sp", g_sb[:], gpost, w=["g"])
    for k in range(8):
        P.dma("pool", wo[:, k, :], w_out[k * 128:(k + 1) * 128, :], w=[("wo", k)])
    mix = [c.sb(f"mix{i}", [128, 8, G], BF16) for i in range(2)]
    xs = [c.sb(f"xs{i}", [128, 8, G], F32) for i in range(2)]
    y = c.sb("y", [128, 8, G], F32)
    sq = c.sb("sq", [128, 8, G], BF16)
    lnv = c.sb("lnv", [128, G], F32)
    rstd = c.sb("rstd", [128, G], F32)
    tt = Ring([c.sb(f"tt{i}", [128, G], F32) for i in range(2)], "tt")
    ot = Ring([c.sb(f"ot{i}", [128, G], F32) for i in range(3)], "ot")
    ps = Ring([c.psum(f"ps{i}", [128, G]) for i in range(8)], "ps")
    mv_ = mixT.rearrange("(k p) t -> p k t", p=128)
    xv = xT.rearrange("(k p) t -> p k t", p=128)
    ov = o_x.rearrange("(k p) t -> p k t", p=128)
    for g in range(NG):
        bi = g % 2
        cols = slice(g * G, (g + 1) * G)
        P.dma("sp", mix[bi][:], mv_[:, :, cols], w=[("mix", bi)])
        P.dma("sp", xs[bi][:], xv[:, :, cols], w=[("xs", bi)])
        for m in range(8):
            pt, pk = ps.next()
            for k in range(8):
                P.op("pe", "matmul", pt[:], wo[:, k, 128 * m:128 * m + 128], mix[bi][:, k, :],
                     start=(k == 0), stop=(k == 7), r=[("wo", k), ("mix", bi)], w=[pk])
            P.op("act", "activation", y[:, m, :], pt[:], AF.Copy, r=[pk], w=[("y", m)])
            P.op("pool", "tensor_tensor", sq[:, m, :], y[:, m, :], y[:, m, :], ALU.mult, r=[("y", m)], w=[("sq", m)])
        pt, pk = ps.next()
        for m in range(8):
            P.op("pe", "matmul", pt[:], ones_bf[:], sq[:, m, :], start=(m == 0), stop=(m == 7),
                 r=["ones_bf", ("sq", m)], w=[pk])
        P.op("act", "activation", lnv[:], pt[:], AF.Ln, scale=1.0 / D, bias=EPS, r=[pk], w=["lnv"])
        P.op("act", "activation", rstd[:], lnv[:], AF.Exp, scale=-0.5, r=["lnv"], w=["rstd"])
        for m in range(8):
            t_, tk = tt.next()
            o_, ok = ot.next()
            P.op("dve", "scalar_tensor_tensor", t_[:], y[:, m, :], g_sb[:, m:m + 1], rstd[:], ALU.mult, ALU.mult,
                 r=[("y", m), "g", "rstd"], w=[tk])
            P.op("pool", "tensor_tensor", o_[:], t_[:], xs[bi][:, m, :], ALU.add, r=[tk, ("xs", bi)], w=[ok])
            P.dma("sp", ov[:, m, cols], o_[:], r=[ok])
    return c.finish()


GF = 256


def build_F():
    c = Ctx()
    nc, P = c.nc, c.P
    xT = c.din("xT", [D, T], F32)
    w_up = c.din("w_up", [D, DFF], F32)
    w_down = c.din("w_down", [DFF, D], F32)
    gpre = c.din("gpre", [128, 8], F32)
    gpost = c.din("gpost", [128, 8], F32)
    o_x = c.dout("x2T", [D, T], F32)
    wu = c.sb("wu", [128, 8, DFF], BF16)
    wd = c.sb("wd", [128, 32, D], BF16)
    g1 = c.sb("g1", [128, 8], F32)
    g2 = c.sb("g2", [128, 8], F32)
    ones_bf = c.sb("ones_bf", [128, 128], BF16)
    P.op("pool", "memset", ones_bf[:], 1.0, w=["ones_bf"])
    P.dma("sp", g1[:], gpre, w=["g1"])
    P.dma("sp", g2[:], gpost, w=["g2"])
    for k in range(8):
        for hf in range(2):
            P.dma("pool", wu[:, k, hf * 2048:(hf + 1) * 2048], w_up[k * 128:(k + 1) * 128, hf * 2048:(hf + 1) * 2048],
                  w=[("wu", k)])
    for f in range(32):
        P.dma("pool", wd[:, f, :], w_down[f * 128:(f + 1) * 128, :], w=[("wd", f)])
    xy = [c.sb(f"xy{i}", [128, 8, GF], F32) for i in range(2)]
    h2 = c.sb("h2", [128, 8, GF], BF16)
    sq = c.sb("sq", [128, 8, GF], BF16)
    u = c.sb("u", [128, 32, GF], BF16)
    lnv = c.sb("lnv", [128, GF], F32)
    rstd = c.sb("rstd", [128, GF], F32)
    rr = Ring([c.sb(f"rr{i}", [128, GF], F32) for i in range(3)], "rr")
    xr = Ring([c.sb(f"xr{i}", [128, GF], F32) for i in range(2)], "xr")
    tt = Ring([c.sb(f"tt{i}", [128, GF], F32) for i in range(2)], "tt")
    ot = Ring([c.sb(f"ot{i}", [128, GF], F32) for i in range(3)], "ot")
    ps = Ring([c.psum(f"ps{i}", [128, G]) for i in range(8)], "ps")
    xv = xT.rearrange("(k p) t -> p k t", p=128)
    ov = o_x.rearrange("(k p) t -> p k t", p=128)
    ngf = T // GF

    def rms(buf, bk, gain, dst):
        for k in range(8):
            if k % 2 == 0:
                P.op("act", "activation", sq[:, k, :], buf[:, k, :], AF.Square, r=[(bk, k)], w=[("sq", k)])
            else:
                P.op("pool", "tensor_tensor", sq[:, k, :], buf[:, k, :], buf[:, k, :], ALU.mult, r=[(bk, k)], w=[("sq", k)])
        pt, pk = ps.next()
        for k in range(8):
            P.op("pe", "matmul", pt[:, 0:GF], ones_bf[:], sq[:, k, :], start=(k == 0), stop=(k == 7),
                 r=["ones_bf", ("sq", k)], w=[pk])
        P.op("act", "activation", lnv[:], pt[:, 0:GF], AF.Ln, scale=1.0 / D, bias=EPS, r=[pk], w=["lnv"])
        P.op("act", "activation", rstd[:], lnv[:], AF.Exp, scale=-0.5, r=["lnv"], w=["rstd"])

    for g in range(ngf):
        bi = g % 2
        cols = slice(g * GF, (g + 1) * GF)
        xb = xy[bi]
        bk = ("xy", bi)
        P.dma("sp", xb[:], xv[:, :, cols], w=[(bk, k) for k in range(8)])
        rms(xb, bk, g1, h2)
        for k in range(8):
            P.op("dve", "scalar_tensor_tensor", h2[:, k, :], xb[:, k, :], g1[:, k:k + 1], rstd[:], ALU.mult, ALU.mult,
                 r=[(bk, k), "g1", "rstd"], w=[("h2", k)])
        for f in range(32):
            pt, pk = ps.next()
            for k in range(8):
                P.op("pe", "matmul", pt[:, 0:GF], wu[:, k, 128 * f:128 * f + 128], h2[:, k, :],
                     start=(k == 0), stop=(k == 7), r=[("wu", k), ("h2", k)], w=[pk])
            r_, rk = rr.next()
            P.op("act", "activation", r_[:], pt[:, 0:GF], AF.Relu, r=[pk], w=[rk])
            eng = "pool" if f % 4 != 3 else "dve"
            P.op(eng, "tensor_tensor", u[:, f, :], r_[:], r_[:], ALU.mult, r=[rk], w=[("u", f)])
        for m in range(8):
            pt, pk = ps.next()
            for f in range(32):
                P.op("pe", "matmul", pt[:, 0:GF], wd[:, f, 128 * m:128 * m + 128], u[:, f, :],
                     start=(f == 0), stop=(f == 31), r=[("wd", f), ("u", f)], w=[pk])
            P.op("act", "activation", xb[:, m, :], pt[:, 0:GF], AF.Copy, r=[pk], w=[(bk, m)])
        rms(xb, bk, g2, None)
        for m in range(8):
            x_, xk = xr.next()
            P.dma("sp", x_[:], xv[:, m, cols], w=[xk])
            t_, tk = tt.next()
            o_, ok = ot.next()
            P.op("dve", "scalar_tensor_tensor", t_[:], xb[:, m, :], g2[:, m:m + 1], rstd[:], ALU.mult, ALU.mult,
                 r=[(bk, m), "g2", "rstd"], w=[tk])
            P.op("pool", "tensor_tensor", o_[:], t_[:], x_[:], ALU.add, r=[tk, xk], w=[ok])
            P.dma("sp", ov[:, m, cols], o_[:], r=[ok])
    return c.finish()


_NC_CACHE = {}


def _get(name, fn):
    if name not in _NC_CACHE:
        _NC_CACHE[name] = fn()
    return _NC_CACHE[name]


def _run(nc, in_maps):
    res = run_bass_kernel_spmd(nc, in_maps, core_ids=list(range(NCORE)))
    return [{k: np.asarray(v) for k, v in r.items()} for r in res.results]


def _pk(g):
    return np.ascontiguousarray(np.asarray(g, dtype=np.float32).reshape(8, 128).T)


def kernel(x, positions, g_mix_pre, w_in, b_igate, b_fgate, g_mlstm_out, w_out,
           g_mix_post, g_mlp_pre, w_up, w_down, g_mlp_post):
    x = np.asarray(x, dtype=np.float32)
    positions = np.asarray(positions).astype(np.int32)
    perm = rope_perm()
    fcs = rope_consts()
    xT = []
    posl = []
    for core in range(NCORE):
        b, j = core // 4, core % 4
        ti = token_index(j)
        xT.append(np.ascontiguousarray(x[b, ti, :].T))
        posl.append(np.ascontiguousarray(positions[b, ti][None, :]))
    for L in range(DEPTH):
        wi = np.asarray(w_in[L], dtype=np.float32)
        wA = np.ascontiguousarray(np.concatenate([wi, wi[:, 0:512][:, perm], wi[:, 512:1024][:, perm]], axis=1))
        gbias = np.ascontiguousarray(np.stack([np.asarray(b_igate[L]), np.asarray(b_fgate[L])], axis=1).astype(np.float32))
        gpre = _pk(g_mix_pre[L])
        outsA = _run(_get("A", build_A), [dict(xT=xT[c_], pos=posl[c_], wA=wA, gpre=gpre, gbias=gbias, fcs=fcs)
                                          for c_ in range(NCORE)])
        outsB = _run(_get("B", build_B), b_inputs(outsA, np.asarray(g_mlstm_out[L], dtype=np.float32)))
        wo = np.ascontiguousarray(np.asarray(w_out[L], dtype=np.float32))
        gpo = _pk(g_mix_post[L])
        outsO = _run(_get("O", build_O), [dict(
            mixT=np.ascontiguousarray(np.concatenate([outsB[c_]["attT"].reshape(512, T), outsB[c_]["mmT"]], axis=0)),
            w_out=wo, xT=xT[c_], gpost=gpo) for c_ in range(NCORE)])
        wu = np.ascontiguousarray(np.asarray(w_up[L], dtype=np.float32))
        wd = np.ascontiguousarray(np.asarray(w_down[L], dtype=np.float32))
        g1, g2 = _pk(g_mlp_pre[L]), _pk(g_mlp_post[L])
        outsF = _run(_get("F", build_F), [dict(xT=outsO[c_]["x1T"], w_up=wu, w_down=wd, gpre=g1, gpost=g2)
                                          for c_ in range(NCORE)])
        xT = [outsF[c_]["x2T"] for c_ in range(NCORE)]
    out = np.zeros((NB, S, D), dtype=np.float32)
    for core in range(NCORE):
        b, j = core // 4, core % 4
        out[b, token_index(j), :] = xT[core].T
    return out
```

```python
import numpy as np
import ml_dtypes
from contextlib import ExitStack
import concourse.bass as bass
import concourse.mybir as mybir
from concourse.bass_utils import run_bass_kernel_spmd

F32 = mybir.dt.float32
BF16 = mybir.dt.bfloat16
I32 = mybir.dt.int32
AF = mybir.ActivationFunctionType
ALU = mybir.AluOpType
AX = mybir.AxisListType
NPBF = ml_dtypes.bfloat16

D = 1024
S = 8192
NB = 2
DEPTH = 2
NCORE = 8
G = 512
NG = 4
T = G * NG
INW = 3080
DFF = 4096
EPS = 1e-6
ROPE_THETA = 500000.0
NEG = -30000.0


def core_groups(j):
    return [j, 7 - j, 8 + j, 15 - j]


class Prog:
    NDMA = 24
    CH = 20000

    def __init__(self, nc, stack, same_engine_sync=True):
        self.nc = nc
        self.stack = stack
        self.ops = []
        self.same = same_engine_sync
        self.engs = {"pe": nc.tensor, "act": nc.scalar, "dve": nc.vector,
                     "pool": nc.gpsimd, "sp": nc.sync}
        self.sems = {}
        self.dsems = [stack.enter_context(nc.semaphore(f"s_dma_{k}")) for k in range(self.NDMA)]
        self.ccsem = stack.enter_context(nc.semaphore("s_cc"))
        self.barsem = stack.enter_context(nc.semaphore("s_bar"))
        self.cnt = {}
        self.waited = {}
        self.dcount = 0
        self.cccount = 0
        self.nbar = 0
        self.dfinal = {}
        self.total_ops = 0

    def op(self, eng, meth, *args, r=(), w=(), **kw):
        self.ops.append(dict(eng=eng, fn=(meth, args, kw), r=tuple(r), w=tuple(w), dma=False, cc=False))

    def dma(self, q, out, in_, r=(), w=(), **kw):
        self.ops.append(dict(eng=q, fn=("dma_start", (), dict(out=out, in_=in_, **kw)),
                             r=tuple(r), w=tuple(w), dma=True, cc=False))

    def allgather(self, src, dst, groups, r=(), w=()):
        self.ops.append(dict(eng="pool", fn=("collective_compute", ("AllGather", ALU.bypass),
                                             dict(replica_groups=groups, ins=[src], outs=[dst])),
                             r=tuple(r), w=tuple(w), dma=True, cc=True))

    def _run(self, o):
        meth, args, kw = o["fn"]
        return getattr(self.engs[o["eng"]], meth)(*args, **kw)

    def _sem(self, e, idx):
        lst = self.sems.setdefault(e, [])
        while len(lst) <= idx:
            lst.append(self.stack.enter_context(self.nc.semaphore(f"s_{e}_{len(lst)}")))
        return lst[idx]

    def _wait(self, eng, sem, val):
        key = (eng, id(sem))
        if self.waited.get(key, 0) < val:
            self.engs[eng].wait_ge(sem, val)
            self.waited[key] = val

    def emit(self):
        ops = self.ops
        self.ops = []
        n = len(ops)
        self.total_ops += n
        last_w = {}
        readers = {}
        deps = [None] * n
        for i, o in enumerate(ops):
            d = set()
            for k in o["r"]:
                if k in last_w:
                    d.add(last_w[k])
            for k in o["w"]:
                if k in last_w:
                    d.add(last_w[k])
                d.update(readers.get(k, ()))
            for k in o["r"]:
                readers.setdefault(k, []).append(i)
            for k in o["w"]:
                last_w[k] = i
                readers[k] = []
            d.discard(i)
            dd = set()
            for j in d:
                oj = ops[j]
                if not oj["dma"] and not o["dma"] and oj["eng"] == o["eng"]:
                    if o["eng"] == "pe" or not self.same:
                        continue
                dd.add(j)
            deps[i] = dd
        needs = [False] * n
        for i in range(n):
            for j in deps[i]:
                needs[j] = True
        lastop = {}
        for i, o in enumerate(ops):
            if not o["dma"]:
                lastop[o["eng"]] = i
        for e, i in lastop.items():
            needs[i] = True
        target = [None] * n
        nd = self.NDMA
        for i, o in enumerate(ops):
            e = o["eng"]
            pend = []

            def need(sem, val):
                key = (e, id(sem))
                if self.waited.get(key, 0) < val:
                    self.waited[key] = val
                    for q in pend:
                        if q[0] is sem:
                            q[1] = val
                            return
                    pend.append([sem, val])

            for j in sorted(deps[i]):
                sem, val = target[j]
                need(sem, val)
            if o["dma"] and not o["cc"]:
                k = self.dcount % nd
                rnd = self.dcount // nd
                if rnd > 0:
                    need(self.dsems[k], 16 * rnd)
            for sem, val in pend[:-1]:
                self.engs[e].wait_ge(sem, val)
            inst = self._run(o)
            if pend:
                inst._wait_ge(pend[-1][0], pend[-1][1])
            if o["cc"]:
                inst.then_inc(self.ccsem)
                self.cccount += 1
                target[i] = (self.ccsem, self.cccount)
            elif o["dma"]:
                inst.then_inc(self.dsems[k], 16)
                target[i] = (self.dsems[k], 16 * (rnd + 1))
                self.dfinal[k] = 16 * (rnd + 1)
                self.dcount += 1
            else:
                if needs[i]:
                    c = self.cnt.get(e, 0)
                    sem = self._sem(e, c // self.CH)
                    inst.then_inc(sem, 1)
                    target[i] = (sem, c % self.CH + 1)
                    self.cnt[e] = c + 1
        for k, v in self.dfinal.items():
            self._wait("sp", self.dsems[k], v)
        if self.cccount:
            self._wait("sp", self.ccsem, self.cccount)
        for e, c in self.cnt.items():
            if c > 0:
                self._wait("sp", self._sem(e, (c - 1) // self.CH), (c - 1) % self.CH + 1)
        sp = self.engs["sp"]
        for k in list(self.dfinal.keys()):
            sp.sem_clear(self.dsems[k])
        for e, c in self.cnt.items():
            for idx in range((c + self.CH - 1) // self.CH):
                sp.sem_clear(self._sem(e, idx))
        if self.cccount:
            sp.sem_clear(self.ccsem)
        self.dfinal = {}
        self.dcount = 0
        self.cccount = 0
        self.cnt = {}
        self.waited = {k: v for k, v in self.waited.items() if k[1] == id(self.barsem)}
        self.nbar += 1
        self.engs["sp"].sem_inc(self.barsem, 1)
        for e in ("pe", "act", "dve", "pool"):
            self._wait(e, self.barsem, self.nbar)


class Ctx:
    def __init__(self):
        self.nc = bass.Bass("TRN2", target_bir_lowering=False)
        self.st = ExitStack()
        self.P = Prog(self.nc, self.st)
        self.ph = None
        self.uid = 0

    def din(self, name, shape, dt):
        return self.nc.dram_tensor(name, list(shape), dt, kind="ExternalInput").ap()

    def dout(self, name, shape, dt):
        return self.nc.dram_tensor(name, list(shape), dt, kind="ExternalOutput").ap()

    def dscr(self, name, shape, dt):
        return self.nc.dram_tensor(name, list(shape), dt).ap()

    def begin(self):
        self.ph = ExitStack()

    def end(self):
        self.P.emit()
        self.ph.close()
        self.ph = None

    def sb(self, name, shape, dt):
        self.uid += 1
        return self.ph.enter_context(self.nc.sbuf_tensor(f"{name}_s{self.uid}", list(shape), dt))

    def psum(self, name, shape, dt=F32):
        self.uid += 1
        return self.ph.enter_context(self.nc.psum_tensor(f"{name}_p{self.uid}", list(shape), dt))

    def finish(self):
        self.st.close()
        return self.nc


class Ring:
    def __init__(self, tiles, name):
        self.tiles = tiles
        self.name = name
        self.i = 0

    def next(self):
        k = self.i % len(self.tiles)
        self.i += 1
        return self.tiles[k], (self.name, k)


NCOLA = INW + 1024
NT_SLOT = [16, 32, 48, 64]
NT_OFF = [0, 16, 48, 96]
NT_ALL = 160
NSEG = 16
GF = 512
GROUPS4 = [[0, 1, 2, 3], [4, 5, 6, 7]]


def owner(gq):
    if gq < 4:
        return gq, 0
    if gq < 8:
        return 7 - gq, 1
    if gq < 12:
        return gq - 8, 2
    return 15 - gq, 3

def phase_A(c, dr, L, x_src):
    nc, P = c.nc, c.P
    c.begin()
    wA, gpre, gbias = dr[f"wA{L}"], dr[f"gpre{L}"], dr[f"gbias{L}"]
    pos, fcs = dr["pos"], dr["fcs"]
    wbf = c.sb("wbf", [128, 8, NCOLA], BF16)
    x_sb = [c.sb(f"x{i}", [128, 8, G], F32) for i in range(2)]
    sq = c.sb("sq", [128, 8, G], BF16)
    hT = c.sb("hT", [128, 8, G], BF16)
    rstd = c.sb("rstd", [128, G], F32)
    lnv = c.sb("lnv", [128, G], F32)
    ones_bf = c.sb("ones_bf", [128, 128], BF16)
    ones4 = c.sb("ones4", [4, G], F32)
    identf = c.sb("identf", [4, 4], F32)
    gpre_sb = c.sb("gpre_sb", [128, 8], F32)
    gb_sb = c.sb("gb_sb", [4, 2], F32)
    ngb_sb = c.sb("ngb_sb", [4, 2], F32)
    fcs_sb = c.sb("fcs_sb", [128, 2], F32)
    posi = c.sb("posi", [128, G], I32)
    posf = c.sb("posf", [128, G], F32)
    yy = c.sb("yy", [128, G], F32)
    nn = c.sb("nn", [128, G], F32)
    Ct = c.sb("Ct", [128, G], F32)
    St = c.sb("St", [128, G], F32)
    t1 = [c.sb(f"t1_{i}", [128, G], F32) for i in range(2)]
    t2 = [c.sb(f"t2_{i}", [128, G], F32) for i in range(2)]
    obf = Ring([c.sb(f"obf{i}", [128, G], BF16) for i in range(6)], "obf")
    o32 = Ring([c.sb(f"o32{i}", [128, G], F32) for i in range(3)], "o32")
    km = c.sb("km", [128, 4, 2 * NG], F32)
    km_bf = c.sb("km_bf", [128, 4, 2 * NG], BF16)
    g4 = [c.sb(f"g4_{i}", [4, G], F32) for i in range(5)]
    vg = Ring([c.sb(f"vg{i}", [128, 8, 4, 65], BF16) for i in range(2)], "vg")
    mk_tok = c.sb("mk_tok", [128, 16, 256], BF16)
    mv_sb = c.sb("mv_sb", [128, 16, 4, 129], BF16)
    ig_tok = c.sb("ig_tok", [128, 16, 4], F32)
    b_tok = c.sb("b_tok", [128, 16, 4], F32)
    brep_own = c.sb("brep_own", [128, 4, 4], F32)
    warg = c.sb("warg", [128, 16, 4], F32)
    wts = c.sb("wts", [128, 16, 4], F32)
    kw = Ring([c.sb(f"kw{i}", [128, 64], BF16) for i in range(4)], "kw")
    cloc_sb = c.sb("cloc_sb", [64, 4, 4, 129], F32)
    ps = Ring([c.psum(f"ps{i}", [128, G]) for i in range(8)], "ps")

    P.op("pool", "memset", ones_bf[:], 1.0, w=["ones_bf"])
    P.op("pool", "memset", ones4[:], 1.0, w=["ones4"])
    P.op("pool", "memset", mv_sb[:], 1.0, w=["mv_sb"])
    for i in range(2):
        P.op("pool", "memset", vg.tiles[i][:], 1.0, w=[("vg", i)])
    P.dma("sp", gpre_sb[:], gpre, w=["gpre"])
    P.dma("sp", gb_sb[:], gbias, w=["gb"])
    P.dma("sp", fcs_sb[:], fcs, w=["fcs"])
    P.dma("sp", identf[:], dr["identf"], w=["identf"])
    P.op("dve", "tensor_scalar", ngb_sb[:], gb_sb[:], -1.0, None, ALU.mult, r=["gb"], w=["ngb"])
    for k in range(8):
        P.dma("pool", wbf[:, k, :], wA[k * 128:(k + 1) * 128, :], w=[("wbf", k)])
    xTv = x_src.rearrange("(k p) t -> p k t", p=128)

    def load_x(g):
        P.dma("sp", x_sb[g % 2][:], xTv[:, :, g * G:(g + 1) * G], w=[("x", g % 2)])

    load_x(0)
    for g in range(NG):
        xs = x_sb[g % 2]
        xk = ("x", g % 2)
        cols = slice(g * G, (g + 1) * G)
        if g + 1 < NG:
            load_x(g + 1)
        P.dma("sp", posi[:], pos[0:1, cols].broadcast_to([128, G]), w=["posi"])
        P.op("dve", "tensor_copy", posf[:], posi[:], r=["posi"], w=["posf"])
        for which, tab in ((0, Ct), (1, St)):
            off = 0.25 if which == 0 else 0.0
            P.op("dve", "tensor_scalar", yy[:], posf[:], fcs_sb[:, which:which + 1], off, ALU.mult, ALU.add,
                 r=["posf", "fcs"], w=["yy"])
            P.op("dve", "tensor_scalar", nn[:], yy[:], 12582912.0, 12582912.0, ALU.add, ALU.subtract,
                 r=["yy"], w=["nn"])
            P.op("dve", "tensor_tensor", yy[:], yy[:], nn[:], ALU.subtract, r=["yy", "nn"], w=["yy"])
            P.op("act", "activation", tab[:], yy[:], AF.Sin, scale=6.28318, r=["yy"], w=[("tab", which)])
        for k in range(8):
            if k % 2 == 0:
                P.op("act", "activation", sq[:, k, :], xs[:, k, :], AF.Square, r=[xk], w=[("sq", k)])
            else:
                P.op("pool", "tensor_tensor", sq[:, k, :], xs[:, k, :], xs[:, k, :], ALU.mult, r=[xk], w=[("sq", k)])
        pt, pk = ps.next()
        for k in range(8):
            P.op("pe", "matmul", pt[:], ones_bf[:], sq[:, k, :], start=(k == 0), stop=(k == 7),
                 r=["ones_bf", ("sq", k)], w=[pk])
        P.op("act", "activation", lnv[:], pt[:], AF.Ln, scale=1.0 / D, bias=EPS, r=[pk], w=["lnv"])
        P.op("act", "activation", rstd[:], lnv[:], AF.Exp, scale=-0.5, r=["lnv"], w=["rstd"])
        for k in range(8):
            P.op("dve", "scalar_tensor_tensor", hT[:, k, :], xs[:, k, :], gpre_sb[:, k:k + 1], rstd[:],
                 ALU.mult, ALU.mult, r=[xk, "gpre", "rstd"], w=[("hT", k)])

        def proj(col0, m):
            pt, pk = ps.next()
            for k in range(8):
                P.op("pe", "matmul", pt[0:m, :], wbf[:, k, col0:col0 + m], hT[:, k, :],
                     start=(k == 0), stop=(k == 7), r=[("wbf", k), ("hT", k)], w=[pk])
            return pt, pk

        def proj_tok(a, col0, n):
            pt, pk = ps.next()
            for k in range(8):
                P.op("pe", "matmul", pt[:, 0:n], hT[:, k, 128 * a:128 * a + 128], wbf[:, k, col0:col0 + n],
                     start=(k == 0), stop=(k == 7), r=[("wbf", k), ("hT", k)], w=[pk])
            return pt, pk

        for qi, (base, pbase, dst) in enumerate(((0, INW, dr["qT"]), (512, INW + 512, None))):
            for ch in range(4):
                pa, pak = proj(base + 128 * ch, 128)
                pb, pbk = proj(pbase + 128 * ch, 128)
                ta, tb = t1[ch % 2], t2[ch % 2]
                P.op("dve", "tensor_tensor", ta[:], pa[:], Ct[:], ALU.mult, r=[pak, ("tab", 0)], w=[("t1", ch % 2)])
                P.op("dve", "tensor_tensor", tb[:], pb[:], St[:], ALU.mult, r=[pbk, ("tab", 1)], w=[("t2", ch % 2)])
                ot, ok = obf.next()
                P.op("pool", "tensor_tensor", ot[:], ta[:], tb[:], ALU.add,
                     r=[("t1", ch % 2), ("t2", ch % 2)], w=[ok])
                if qi == 0:
                    P.dma("sp", dst[128 * ch:128 * (ch + 1), cols], ot[:], r=[ok])
                else:
                    P.dma("sp", dr[f"kT_src{ch}"][:, cols], ot[:], r=[ok])
                if qi == 1:
                    P.op("dve", "reduce_sum", km[:, ch, 2 * g:2 * g + 2],
                         ot[:].rearrange("p (b t) -> p b t", b=2), AX.X, r=[ok], w=[("km", ch)])
        plain = [(1536 + 128 * i, dr["mqT"], i, 1.0) for i in range(2)]
        plain += [(1792 + 128 * i, dr["mkT"], i, 0.125) for i in range(2)]
        for n_, (col0, dst, ch, scl) in enumerate(plain):
            pa, pak = proj(col0, 128)
            ot, ok = obf.next()
            if n_ % 2 == 0:
                P.op("act", "activation", ot[:], pa[:], AF.Copy, scale=scl, r=[pak], w=[ok])
            else:
                P.op("dve", "tensor_scalar", ot[:], pa[:], scl, None, ALU.mult, r=[pak], w=[ok])
            P.dma("sp", dst[128 * ch:128 * (ch + 1), cols], ot[:], r=[ok])
        for ch in range(4):
            pa, pak = proj(2560 + 128 * ch, 128)
            ot, ok = o32.next()
            P.op("act", "activation", ot[:], pa[:], AF.Sigmoid, r=[pak], w=[ok])
            P.dma("sp", dr["sgT"][128 * ch:128 * (ch + 1), cols], ot[:], r=[ok])
        vt, vk = vg.next()
        for a in range(4):
            t = 4 * g + a
            pa, pak = proj_tok(a, 1024, 512)
            P.op("act", "activation", vt[:, :, a, 0:64], pa[:].rearrange("p (h d) -> p h d", h=8), AF.Copy,
                 r=[pak], w=[vk])
            pa, pak = proj_tok(a, 1792, 256)
            P.op("dve", "tensor_scalar", mk_tok[:, t, :], pa[:, 0:256], 0.125, None, ALU.mult,
                 r=[pak], w=[("mk_tok", t)])
            pa, pak = proj_tok(a, 2048, 512)
            P.op("act", "activation", mv_sb[:, t, :, 0:128], pa[:].rearrange("p (h v) -> p h v", h=4), AF.Copy,
                 r=[pak, "mv_sb"], w=[("mv_sb", t)])
        for c4 in range(4):
            P.dma("sp", dr[f"vsrc{c4}"][:, :, 4 * g:4 * g + 4, :], vt[:, 2 * c4:2 * c4 + 2, :, :], r=[vk])
        pa, pak = proj(3072, 4)
        P.op("dve", "tensor_scalar", g4[0][:], pa[0:4, :], gb_sb[:, 0:1], None, ALU.add, r=[pak, "gb"], w=[("g4", 0)])
        pa, pak = proj(3076, 4)
        P.op("act", "activation", g4[1][:], pa[0:4, :], AF.Exp, scale=-1.0, bias=ngb_sb[:, 1:2],
             r=[pak, "ngb"], w=[("g4", 1)])
        P.op("act", "activation", g4[2][:], g4[1][:], AF.Ln, bias=1.0, r=[("g4", 1)], w=[("g4", 2)])
        P.op("dve", "tensor_scalar", g4[3][:], g4[2][:], -1.0, None, ALU.mult, r=[("g4", 2)], w=[("g4", 3)])
        P.op("dve", "tensor_tensor_scan", g4[4][:], ones4[:], g4[3][:], 0.0, ALU.mult, ALU.add,
             r=["ones4", ("g4", 3)], w=[("g4", 4)])
        P.dma("sp", dr["bT"][:, cols], g4[4][:], r=[("g4", 4)])
        P.dma("sp", dr["blast_src"][0:1, 4 * g:4 * g + 4].rearrange("o h -> h o"), g4[4][0:4, G - 1:G],
              r=[("g4", 4)], w=["blast_src"])
        for a in range(4):
            for src_i, dstt, dk in ((0, ig_tok, "ig_tok"), (4, b_tok, "b_tok")):
                pa, pak = ps.next()
                P.op("pe", "matmul", pa[:, 0:4], g4[src_i][0:4, 128 * a:128 * a + 128], identf[:],
                     start=True, stop=True, r=[("g4", src_i), "identf"], w=[pak])
                P.op("dve", "tensor_copy", dstt[:, 4 * g + a, :], pa[:, 0:4], r=[pak], w=[(dk, 4 * g + a)])
    for ch in range(4):
        P.op("dve", "tensor_scalar", km_bf[:, ch, :], km[:, ch, :], 1.0 / 256, None, ALU.mult,
             r=[("km", ch)], w=[("kmb", ch)])
        P.dma("sp", dr["km_src"][128 * ch:128 * (ch + 1), :], km_bf[:, ch, :], r=[("kmb", ch)])
    igk = [("ig_tok", t) for t in range(16)]
    bk_ = [("b_tok", t) for t in range(16)]
    P.dma("sp", brep_own[:].rearrange("p l h -> p (l h)"), dr["blast_src"][0:1, :].broadcast_to([128, 16]),
          r=["blast_src"], w=["brep_own"])
    P.op("dve", "tensor_tensor", warg[:], ig_tok[:], b_tok[:], ALU.subtract, r=igk + bk_, w=["warg"])
    for l in range(4):
        P.op("dve", "tensor_tensor", warg[:, 4 * l:4 * l + 4, :], warg[:, 4 * l:4 * l + 4, :],
             brep_own[:, l:l + 1, :].broadcast_to([128, 4, 4]), ALU.add, r=["warg", "brep_own"], w=["warg"])
    P.op("act", "activation", wts[:], warg[:], AF.Exp, r=["warg"], w=["wts"])
    for l in range(4):
        for h in range(4):
            pt, pk = ps.next()
            for a in range(4):
                t = 4 * l + a
                kt, kk = kw.next()
                eng = "dve" if (a % 2 == 0) else "pool"
                P.op(eng, "tensor_scalar", kt[:], mk_tok[:, t, 64 * h:64 * h + 64], wts[:, t, h:h + 1], None, ALU.mult,
                     r=[("mk_tok", t), "wts"], w=[kk])
                P.op("pe", "matmul", pt[0:64, 0:129], kt[:], mv_sb[:, t, h, :], start=(a == 0), stop=(a == 3),
                     r=[kk, ("mv_sb", t)], w=[pk])
            P.op("act", "activation", cloc_sb[:, l, h, :], pt[0:64, 0:129], AF.Copy, r=[pk], w=["cloc_sb"])
    P.dma("sp", dr["cloc_src"], cloc_sb[:].rearrange("p l h c -> p (l h c)"), r=["cloc_sb"])
    P.dma("sp", dr["igo"], ig_tok[:], r=igk)
    P.dma("sp", dr["bo"], b_tok[:], r=bk_)
    P.dma("sp", dr["mvo"], mv_sb[:], r=[("mv_sb", t) for t in range(16)])
    c.end()


def phase_CC(c, dr):
    P = c.P
    c.begin()
    for i in range(4):
        P.allgather(dr[f"kT_src{i}"], dr[f"kT_all{i}"], GROUPS4)
    for i in range(4):
        P.allgather(dr[f"vsrc{i}"].rearrange("p h t c -> p (h t c)"), dr[f"v_all{i}"], GROUPS4)
    P.allgather(dr["km_src"], dr["km_all"], GROUPS4)
    P.allgather(dr["cloc_src"], dr["cloc_all"], GROUPS4)
    P.allgather(dr["blast_src"], dr["blast_all"], GROUPS4)
    c.end()


def phase_B(c, dr, L):
    nc, P = c.nc, c.P
    c.begin()
    qT = dr["qT"]
    v_all = [dr[f"v_all{i}"].rearrange("(j p) (h t c) -> j p h t c", p=128, h=2, t=16) for i in range(4)]
    mixT = dr["mixT"]
    psb = [c.psum(f"ps{i}", [128, G]) for i in range(8)]
    rA = Ring(psb[0:3], "psA")
    rB = Ring(psb[3:5], "psB")
    rC = Ring(psb[5:8], "psC")
    ones_bf = c.sb("ones_bf", [128, 128], BF16)
    ones_f = c.sb("ones_f", [128, 128], F32)
    P.op("pool", "memset", ones_bf[:], 1.0, w=["ones_bf"])
    P.op("pool", "memset", ones_f[:], 1.0, w=["ones_f"])
    ident_sb = c.sb("ident_sb", [128, 128], BF16)
    tri_sb = c.sb("tri_sb", [128, 128], BF16)
    trif_sb = c.sb("trif_sb", [128, 128], F32)
    P.dma("sp", ident_sb[:], dr["ident"], w=["ident"])
    P.dma("sp", tri_sb[:], dr["tri"], w=["tri"])
    P.dma("sp", trif_sb[:], dr["trif"], w=["trif"])

    brep_sb = c.sb("brep_sb", [128, NSEG, 4], F32)
    eB = c.sb("eB", [128, NSEG, 4], F32)
    mcol_sb = c.sb("mcol_sb", [64, 4, NSEG], F32)
    gml_sb = c.sb("gml_sb", [128, 4], F32)
    cloc = c.sb("cloc", [64, NSEG, 4, 129], F32)
    P.dma("sp", mcol_sb[:], dr["mcol"], w=["mcol"])
    P.dma("sp", gml_sb[:], dr[f"gml{L}"], w=["gml"])
    for gq in range(NSEG):
        j_, l_ = owner(gq)
        P.dma("sp", brep_sb[:, gq, :], dr["blast_all"][j_:j_ + 1, 4 * l_:4 * l_ + 4].broadcast_to([128, 4]), w=["brep"])
        P.dma("sp", cloc[:, gq, :, :].rearrange("p h c -> p (h c)"),
              dr["cloc_all"][64 * j_:64 * j_ + 64, 516 * l_:516 * l_ + 516], w=[("cloc", gq)])
    P.op("act", "activation", eB[:], brep_sb[:], AF.Exp, r=["brep"], w=["eB"])
    eBm = c.sb("eBm", [64, 4, NSEG, 4], F32)
    for l in range(4):
        for h in range(4):
            P.op("dve", "scalar_tensor_tensor", eBm[:, l, :, h], eB[0:64, :, h], -1.0, mcol_sb[:, l, :],
                 ALU.add, ALU.mult, r=["eB", "mcol"], w=[("eBm", l, h)])
            P.op("dve", "tensor_scalar", eBm[:, l, :, h], eBm[:, l, :, h], 1.0, None, ALU.add,
                 r=[("eBm", l, h)], w=[("eBm", l, h)])
    st_ = [c.sb(f"st{l}", [64, 4, 129], F32) for l in range(4)]
    tmpS = Ring([c.sb(f"tmpS{i}", [64, 4, 129], F32) for i in range(2)], "tmpS")
    cin_bf = [[c.sb(f"cin{l}_{h}", [64, 128], BF16) for h in range(4)] for l in range(4)]
    nin_bf = [[c.sb(f"nin{l}_{h}", [64, 128], BF16) for h in range(4)] for l in range(4)]
    for l in range(4):
        S_ = st_[l]
        sk = ("st", l)
        P.op("pool", "memset", S_[:], 0.0, w=[sk])
        for s_ in range(NSEG - 1):
            tt, tk = tmpS.next()
            P.op("dve", "tensor_tensor", tt[:], S_[:], eBm[:, l, s_, :].unsqueeze(2).broadcast_to([64, 4, 129]),
                 ALU.mult, r=[sk] + [("eBm", l, h) for h in range(4)], w=[tk])
            P.op("dve", "scalar_tensor_tensor", S_[:].rearrange("p h c -> p (h c)"),
                 cloc[:, s_, :, :].rearrange("p h c -> p (h c)"), mcol_sb[:, l, s_:s_ + 1],
                 tt[:].rearrange("p h c -> p (h c)"), ALU.mult, ALU.add, r=[("cloc", s_), "mcol", tk], w=[sk])
        for h in range(4):
            P.op("act", "activation", cin_bf[l][h][:], S_[:, h, 0:128], AF.Copy, r=[sk], w=[("cin", l, h)])
            P.op("dve", "tensor_copy", nin_bf[l][h][:], S_[:, h, 128:129].broadcast_to([64, 128]),
                 r=[sk], w=[("nin", l, h)])
    igo_sb = c.sb("igo_sb", [128, 16, 4], F32)
    bo_sb = c.sb("bo_sb", [128, 16, 4], F32)
    negc = c.sb("negc", [128, 16, 4], F32)
    P.dma("sp", igo_sb[:], dr["igo"], w=["igo"])
    P.dma("sp", bo_sb[:], dr["bo"], w=["bo"])
    P.op("dve", "tensor_tensor", negc[:], igo_sb[:], bo_sb[:], ALU.subtract, r=["igo", "bo"], w=["negc"])
    mvo_sb = c.sb("mvo_sb", [128, 16, 4, 129], BF16)
    P.dma("sp", mvo_sb[:], dr["mvo"], w=["mvo"])
    mq_t = Ring([c.sb(f"mq_t{i}", [64, G], BF16) for i in range(2)], "mq_t")
    mk_t = Ring([c.sb(f"mk_t{i}", [64, G], BF16) for i in range(2)], "mk_t")
    sg_t = Ring([c.sb(f"sg_t{i}", [128, G], F32) for i in range(2)], "sg_t")
    br_t = Ring([c.sb(f"br_t{i}", [1, G], F32) for i in range(2)], "br_t")
    EB = c.sb("EB", [128, G], F32)
    Et = Ring([c.sb(f"Et{i}", [128, G], F32) for i in range(2)], "Et")
    Pm = Ring([c.sb(f"Pm{i}", [128, G], BF16) for i in range(2)], "Pm")
    QE = c.sb("QE", [64, G], BF16)
    dn = c.sb("dn", [128, G], F32)
    rdn = c.sb("rdn", [128, G], F32)
    hh = c.sb("hh", [128, G], F32)
    hsq = c.sb("hsq", [128, G], BF16)
    lnh = c.sb("lnh", [128, G], F32)
    rsh = c.sb("rsh", [128, G], F32)
    hn = c.sb("hn", [128, G], F32)
    mo_t = Ring([c.sb(f"mo_t{i}", [128, G], BF16) for i in range(2)], "mo_t")
    def ml_block(l, h):
        cols = slice(l * G, (l + 1) * G)
        mq, mqk = mq_t.next()
        mk, mkk = mk_t.next()
        sgt, sgk = sg_t.next()
        brt, brk = br_t.next()
        P.dma("sp", mq[:], dr["mqT"][64 * h:64 * h + 64, cols], w=[mqk])
        P.dma("sp", mk[:], dr["mkT"][64 * h:64 * h + 64, cols], w=[mkk])
        P.dma("sp", sgt[:], dr["sgT"][128 * h:128 * h + 128, cols], w=[sgk])
        P.dma("sp", brt[:], dr["bT"][h:h + 1, cols], w=[brk])
        bb, bbk = rA.next()
        P.op("pe", "matmul", bb[:], ones_f[0:1, :], brt[:], start=True, stop=True, r=["ones_f", brk], w=[bbk])
        P.op("act", "activation", EB[:], bb[:], AF.Exp, r=[bbk], w=["EB"])
        num, numk = rB.next()
        den, denk = rB.next()
        for a in range(4):
            n0 = 128 * a
            et, ek = Et.next()
            P.op("act", "activation", et[:, n0:G], bb[:, n0:G], AF.Exp, bias=negc[:, 4 * l + a, h:h + 1],
                 r=[bbk, "negc"], w=[ek])
            P.op("pool", "tensor_tensor", et[:, n0:n0 + 128], et[:, n0:n0 + 128], trif_sb[:], ALU.mult,
                 r=[ek, "trif"], w=[ek])
            gm, gmk = rC.next()
            P.op("pe", "matmul", gm[:, n0:G], mk[:, n0:n0 + 128], mq[:, n0:G], start=True, stop=True,
                 r=[mkk, mqk], w=[gmk])
            pm, pmk = Pm.next()
            P.op("dve", "tensor_tensor", pm[:, n0:G], gm[:, n0:G], et[:, n0:G], ALU.mult, r=[gmk, ek], w=[pmk])
            P.op("pe", "matmul", num[:, n0:G], mvo_sb[:, 4 * l + a, h, 0:128], pm[:, n0:G],
                 start=(a == 0), stop=False, r=["mvo", pmk], w=[numk])
            P.op("pe", "matmul", den[:, n0:G], ones_bf[:], pm[:, n0:G],
                 start=(a == 0), stop=False, r=["ones_bf", pmk], w=[denk])
        P.op("pool", "tensor_tensor", QE[:], mq[:], EB[0:64, :], ALU.mult, r=[mqk, "EB"], w=["QE"])
        P.op("pe", "matmul", num[:], cin_bf[l][h][:], QE[:], start=False, stop=True,
             r=[("cin", l, h), "QE"], w=[numk])
        P.op("pe", "matmul", den[:], nin_bf[l][h][:], QE[:], start=False, stop=True,
             r=[("nin", l, h), "QE"], w=[denk])
        P.op("act", "activation", dn[:], den[:], AF.Abs, r=[denk], w=["dn"])
        P.op("dve", "tensor_scalar", dn[:], dn[:], 1.0, None, ALU.max, r=["dn"], w=["dn"])
        P.op("dve", "reciprocal", rdn[:], dn[:], r=["dn"], w=["rdn"])
        P.op("dve", "tensor_tensor", hh[:], num[:], rdn[:], ALU.mult, r=[numk, "rdn"], w=["hh"])
        P.op("pool", "tensor_tensor", hsq[:], hh[:], hh[:], ALU.mult, r=["hh"], w=["hsq"])
        ss, ssk = rA.next()
        P.op("pe", "matmul", ss[:], ones_bf[:], hsq[:], start=True, stop=True, r=["ones_bf", "hsq"], w=[ssk])
        P.op("act", "activation", lnh[:], ss[:], AF.Ln, scale=1.0 / 128, bias=EPS, r=[ssk], w=["lnh"])
        P.op("act", "activation", rsh[:], lnh[:], AF.Exp, scale=-0.5, r=["lnh"], w=["rsh"])
        P.op("dve", "tensor_tensor", hn[:], hh[:], rsh[:], ALU.mult, r=["hh", "rsh"], w=["hn"])
        mo, mok = mo_t.next()
        P.op("dve", "scalar_tensor_tensor", mo[:], hn[:], gml_sb[:, h:h + 1], sgt[:], ALU.mult, ALU.mult,
             r=["hn", "gml", sgk], w=[mok])
        P.dma("sp", mixT[512 + 128 * h:512 + 128 * h + 128, cols], mo[:], r=[mok])


    gmask_sb = c.sb("gmask_sb", [128, 16, 32], F32)
    mask2_sb = c.sb("mask2_sb", [128, 16, 32], F32)
    P.dma("sp", gmask_sb[:], dr["gmask"], w=["gmask"])
    P.dma("sp", mask2_sb[:], dr["mask2"], w=["mask2"])
    kaug = [c.sb(f"kaug{i}", [96, 64 * 128], BF16) for i in range(2)]
    vaug = [c.sb(f"vaug{i}", [128, 64, 65], BF16) for i in range(2)]
    qaug = [c.sb(f"qaug{i}", [96, G], BF16) for i in range(2)]
    km_sb = c.sb("km_sb", [64, 8, 32], BF16)
    for gq in range(NSEG):
        j_, l_ = owner(gq)
        P.dma("sp", km_sb[:, :, 2 * gq:2 * gq + 2],
              dr["km_all"][512 * j_:512 * j_ + 512, 2 * l_:2 * l_ + 2].rearrange("(h p) n -> p h n", p=64), w=["km"])
    gm_s = c.sb("gm_s", [128, 32], F32)
    m8 = c.sb("m8", [128, 8], F32)
    mbf = c.sb("mbf", [128, 32], F32)
    mbq_r = Ring([c.sb(f"mbq{i}", [128, 96], BF16) for i in range(2)], "mbq")
    for i in range(2):
        P.op("pool", "memset", mbq_r.tiles[i][:], 0.0, w=[("mbq", i)])
    pT = Ring([c.sb(f"pT{i}", [128, G], BF16) for i in range(4)], "pT")
    dn_a = c.sb("dn_a", [128, G], F32)
    bc_a = c.sb("bc_a", [64, G], F32)
    ao = Ring([c.sb(f"ao{i}", [64, G], BF16) for i in range(2)], "ao")
    iters = [(h, l) for h in range(8) for l in range(4)]
    ml_blocks = [(l, h) for l in range(4) for h in range(4)]

    def bufs(it):
        bi = it % 2
        return kaug[bi], vaug[bi], qaug[bi], ("kaug", bi), ("vaug", bi), ("qaug", bi)

    def loads(it):
        h, l = iters[it]
        nt = NT_SLOT[l]
        k0 = NT_OFF[l] * 128
        cols = slice(l * G, (l + 1) * G)
        ka, va, qa, kk_, vk_, qk_ = bufs(it)
        hc, hr = h // 2, h % 2
        P.dma("sp", qa[0:64, :], qT[64 * h:64 * h + 64, cols], w=[qk_])
        P.dma("sp", ka[0:64, 0:G], dr[f"kT_src{hc}"][64 * hr:64 * hr + 64, cols], w=[kk_])
        P.dma("sp", va[:, 0:4, :], dr[f"vsrc{hc}"][:, hr, 4 * l:4 * l + 4, :], w=[vk_])
        P.dma("sp", ka[64:96, 0:nt * 128], dr["eh"][:, k0:k0 + nt * 128], w=[kk_])
        for gq in range((nt - 4) // 4):
            j_, l_ = owner(gq)
            P.dma("sp", ka[0:64, G * (gq + 1):G * (gq + 2)],
                  dr[f"kT_all{hc}"][128 * j_ + 64 * hr:128 * j_ + 64 * hr + 64, l_ * G:(l_ + 1) * G], w=[kk_])
            P.dma("sp", va[:, 4 * (gq + 1):4 * (gq + 2), :], v_all[hc][j_, :, hr, 4 * l_:4 * l_ + 4, :], w=[vk_])

    gate_state = {}

    def gating_a(it, qt):
        h, l = iters[it]
        ka, va, qa, kk_, vk_, qk_ = bufs(it)
        gp, gpk = rC.next()
        P.op("pe", "matmul", gp[:, 0:32], qa[0:64, 128 * qt:128 * qt + 128], km_sb[:, h, :],
             start=True, stop=True, r=[qk_, "km"], w=[gpk])
        P.op("dve", "tensor_tensor", gm_s[:], gp[:, 0:32], gmask_sb[:, 4 * l + qt, :], ALU.add,
             r=[gpk, "gmask"], w=["gm_s"])
        P.op("dve", "max", m8[:], gm_s[:], r=["gm_s"], w=["m8"])
        P.op("dve", "tensor_tensor", mbf[:], gm_s[:], m8[:, 3:4].broadcast_to([128, 32]), ALU.is_lt,
             r=["gm_s", "m8"], w=["mbf"])
        mq_, mqk_ = mbq_r.next()
        P.op("dve", "scalar_tensor_tensor", mq_[:, 64:96], mbf[:], NEG, mask2_sb[:, 4 * l + qt, :],
             ALU.mult, ALU.add, r=["mbf", "mask2"], w=[mqk_])
        gate_state[(it, qt)] = (mq_, mqk_)

    def gating_b(it, qt):
        ka, va, qa, kk_, vk_, qk_ = bufs(it)
        mq_, mqk_ = gate_state.pop((it, qt))
        tp, tpk = rC.next()
        P.op("pe", "matmul", tp[0:96, 0:128], mq_[:], ident_sb[:], start=True, stop=True,
             r=[mqk_, "ident"], w=[tpk])
        P.op("act", "activation", qa[64:96, 128 * qt:128 * qt + 128], tp[64:96, 0:128], AF.Copy,
             r=[tpk], w=[qk_])

    def tail(it, acc, acck):
        h, l = iters[it]
        cols = slice(l * G, (l + 1) * G)
        P.op("act", "activation", dn_a[64:65, :], acc[64:65, :], AF.Copy, r=[acck], w=["dn_a"])
        P.op("dve", "reciprocal", dn_a[64:65, :], dn_a[64:65, :], r=["dn_a"], w=["dn_a"])
        bcp, bcpk = rC.next()
        P.op("pe", "matmul", bcp[0:64, :], ones_f[64:65, 0:64], dn_a[64:65, :], start=True, stop=True,
             r=["ones_f", "dn_a"], w=[bcpk])
        P.op("act", "activation", bc_a[:], bcp[0:64, :], AF.Copy, r=[bcpk], w=["bc_a"])
        at, atk = ao.next()
        P.op("dve", "tensor_tensor", at[:], acc[0:64, :], bc_a[:], ALU.mult, r=[acck, "bc_a"], w=[atk])
        P.dma("sp", mixT[64 * h:64 * h + 64, cols], at[:], r=[atk])

    nit = len(iters)
    loads(0)
    for qt in range(4):
        gating_a(0, qt)
        gating_b(0, qt)
    loads(1)
    SKEW = 2
    for it in range(nit):
        h, l = iters[it]
        nt = NT_SLOT[l]
        ka, va, qa, kk_, vk_, qk_ = bufs(it)
        acc, acck = rB.next()
        pend = []

        def pv(item):
            kt, n0, pt, ptk = item
            P.op("pe", "matmul", acc[0:65, n0:G], va[:, kt, :], pt[:, n0:G],
                 start=(kt == 0), stop=(kt == nt - 1), r=[vk_, ptk], w=[acck])

        for kt in range(nt):
            n0 = 128 * kt if kt < 4 else 0
            sp_, spk = rA.next()
            P.op("pe", "matmul", sp_[:, n0:G], ka[:, 128 * kt:128 * kt + 128], qa[:, n0:G],
                 start=True, stop=True, r=[kk_, qk_], w=[spk])
            pt, ptk = pT.next()
            P.op("act", "activation", pt[:, n0:G], sp_[:, n0:G], AF.Exp, scale=0.125, r=[spk], w=[ptk])
            if kt < 4:
                P.op("pool", "tensor_tensor", pt[:, n0:n0 + 128], pt[:, n0:n0 + 128], tri_sb[:], ALU.mult,
                     r=[ptk, "tri"], w=[ptk])
            pend.append((kt, n0, pt, ptk))
            if len(pend) > SKEW:
                pv(pend.pop(0))
            if it + 1 < nit and kt >= 4:
                s_ = kt - 4
                if s_ % 3 == 0 and s_ // 3 < 4:
                    gating_a(it + 1, s_ // 3)
                if s_ % 3 == 2 and s_ // 3 < 4:
                    gating_b(it + 1, s_ // 3)
        while pend:
            pv(pend.pop(0))
        tail(it, acc, acck)
        if it + 2 < nit:
            loads(it + 2)
        if it % 2 == 1 and ml_blocks:
            ml_block(*ml_blocks.pop(0))
    while ml_blocks:
        ml_block(*ml_blocks.pop(0))
    c.end()


def phase_O(c, dr, L, x_src, x_dst):
    nc, P = c.nc, c.P
    c.begin()
    wo = c.sb("wo", [128, 8, D], BF16)
    g_sb = c.sb("g_sb", [128, 8], F32)
    ones_bf = c.sb("ones_bf", [128, 128], BF16)
    P.op("pool", "memset", ones_bf[:], 1.0, w=["ones_bf"])
    P.dma("sp", g_sb[:], dr[f"gpost{L}"], w=["g"])
    for k in range(8):
        P.dma("pool", wo[:, k, :], dr[f"w_out{L}"][k * 128:(k + 1) * 128, :], w=[("wo", k)])
    mix = [c.sb(f"mix{i}", [128, 8, G], BF16) for i in range(2)]
    xs = [c.sb(f"xs{i}", [128, 8, G], F32) for i in range(2)]
    y = c.sb("y", [128, 8, G], F32)
    sq = c.sb("sq", [128, 8, G], BF16)
    lnv = c.sb("lnv", [128, G], F32)
    rstd = c.sb("rstd", [128, G], F32)
    tt = Ring([c.sb(f"tt{i}", [128, G], F32) for i in range(2)], "tt")
    ot = Ring([c.sb(f"ot{i}", [128, G], F32) for i in range(3)], "ot")
    ps = Ring([c.psum(f"ps{i}", [128, G]) for i in range(8)], "ps")
    mv_ = dr["mixT"].rearrange("(k p) t -> p k t", p=128)
    xv = x_src.rearrange("(k p) t -> p k t", p=128)
    ov = x_dst.rearrange("(k p) t -> p k t", p=128)
    for g in range(NG):
        bi = g % 2
        cols = slice(g * G, (g + 1) * G)
        P.dma("sp", mix[bi][:], mv_[:, :, cols], w=[("mix", bi)])
        P.dma("sp", xs[bi][:], xv[:, :, cols], w=[("xs", bi)])
        for m in range(8):
            pt, pk = ps.next()
            for k in range(8):
                P.op("pe", "matmul", pt[:], wo[:, k, 128 * m:128 * m + 128], mix[bi][:, k, :],
                     start=(k == 0), stop=(k == 7), r=[("wo", k), ("mix", bi)], w=[pk])
            P.op("act", "activation", y[:, m, :], pt[:], AF.Copy, r=[pk], w=[("y", m)])
            P.op("pool", "tensor_tensor", sq[:, m, :], y[:, m, :], y[:, m, :], ALU.mult, r=[("y", m)], w=[("sq", m)])
        pt, pk = ps.next()
        for m in range(8):
            P.op("pe", "matmul", pt[:], ones_bf[:], sq[:, m, :], start=(m == 0), stop=(m == 7),
                 r=["ones_bf", ("sq", m)], w=[pk])
        P.op("act", "activation", lnv[:], pt[:], AF.Ln, scale=1.0 / D, bias=EPS, r=[pk], w=["lnv"])
        P.op("act", "activation", rstd[:], lnv[:], AF.Exp, scale=-0.5, r=["lnv"], w=["rstd"])
        for m in range(8):
            t_, tk = tt.next()
            o_, ok = ot.next()
            P.op("dve", "scalar_tensor_tensor", t_[:], y[:, m, :], g_sb[:, m:m + 1], rstd[:], ALU.mult, ALU.mult,
                 r=[("y", m), "g", "rstd"], w=[tk])
            P.op("pool", "tensor_tensor", o_[:], t_[:], xs[bi][:, m, :], ALU.add, r=[tk, ("xs", bi)], w=[ok])
            P.dma("sp", ov[:, m, cols], o_[:], r=[ok])
    c.end()


def phase_F(c, dr, L, x_src, x_dst):
    nc, P = c.nc, c.P
    c.begin()
    wu = c.sb("wu", [128, 8, DFF], BF16)
    wd = c.sb("wd", [128, 32, D], BF16)
    g1 = c.sb("g1", [128, 8], F32)
    g2 = c.sb("g2", [128, 8], F32)
    ones_bf = c.sb("ones_bf", [128, 128], BF16)
    P.op("pool", "memset", ones_bf[:], 1.0, w=["ones_bf"])
    P.dma("sp", g1[:], dr[f"g1_{L}"], w=["g1"])
    P.dma("sp", g2[:], dr[f"g2_{L}"], w=["g2"])
    w_up, w_down = dr[f"w_up{L}"], dr[f"w_down{L}"]
    for hf in range(4):
        for k in range(8):
            P.dma("pool", wu[:, k, hf * 1024:(hf + 1) * 1024], w_up[k * 128:(k + 1) * 128, hf * 1024:(hf + 1) * 1024],
                  w=[("wu", k, hf)])
    for f in range(32):
        P.dma("pool", wd[:, f, :], w_down[f * 128:(f + 1) * 128, :], w=[("wd", f)])
    xy = [c.sb("xy0", [128, 8, GF], F32)]
    sq = c.sb("hs", [128, 8, GF], BF16)
    h2 = sq
    u = c.sb("u", [128, 32, GF], BF16)
    lnv = c.sb("lnv", [128, GF], F32)
    rstd = c.sb("rstd", [128, GF], F32)
    rr = Ring([c.sb(f"rr{i}", [128, GF], F32) for i in range(2)], "rr")
    xr = Ring([c.sb(f"xr{i}", [128, GF], F32) for i in range(2)], "xr")
    tt = Ring([c.sb(f"tt{i}", [128, GF], F32) for i in range(1)], "tt")
    ot = Ring([c.sb(f"ot{i}", [128, GF], F32) for i in range(2)], "ot")
    ps = Ring([c.psum(f"ps{i}", [128, G]) for i in range(8)], "ps")
    xv = x_src.rearrange("(k p) t -> p k t", p=128)
    ov = x_dst.rearrange("(k p) t -> p k t", p=128)

    def rms(buf, bk):
        for k in range(8):
            if k % 2 == 0:
                P.op("act", "activation", sq[:, k, :], buf[:, k, :], AF.Square, r=[(bk, k)], w=[("hs", k)])
            else:
                P.op("pool", "tensor_tensor", sq[:, k, :], buf[:, k, :], buf[:, k, :], ALU.mult, r=[(bk, k)], w=[("hs", k)])
        pt, pk = ps.next()
        for k in range(8):
            P.op("pe", "matmul", pt[:, 0:GF], ones_bf[:], sq[:, k, :], start=(k == 0), stop=(k == 7),
                 r=["ones_bf", ("hs", k)], w=[pk])
        P.op("act", "activation", lnv[:], pt[:, 0:GF], AF.Ln, scale=1.0 / D, bias=EPS, r=[pk], w=["lnv"])
        P.op("act", "activation", rstd[:], lnv[:], AF.Exp, scale=-0.5, r=["lnv"], w=["rstd"])

    for g in range(T // GF):
        bi = 0
        cols = slice(g * GF, (g + 1) * GF)
        xb = xy[bi]
        bk = ("xy", bi)
        P.dma("sp", xb[:], xv[:, :, cols], w=[(bk, k) for k in range(8)])
        rms(xb, bk)
        for k in range(8):
            P.op("dve", "scalar_tensor_tensor", h2[:, k, :], xb[:, k, :], g1[:, k:k + 1], rstd[:], ALU.mult, ALU.mult,
                 r=[(bk, k), "g1", "rstd"], w=[("hs", k)])
        for f in range(32):
            pt, pk = ps.next()
            for k in range(8):
                P.op("pe", "matmul", pt[:, 0:GF], wu[:, k, 128 * f:128 * f + 128], h2[:, k, :],
                     start=(k == 0), stop=(k == 7), r=[("wu", k, f // 8), ("hs", k)], w=[pk])
            r_, rk = rr.next()
            P.op("act", "activation", r_[:], pt[:, 0:GF], AF.Relu, r=[pk], w=[rk])
            eng = "pool" if f % 4 != 3 else "dve"
            P.op(eng, "tensor_tensor", u[:, f, :], r_[:], r_[:], ALU.mult, r=[rk], w=[("u", f)])
        for m in range(8):
            pt, pk = ps.next()
            for f in range(32):
                P.op("pe", "matmul", pt[:, 0:GF], wd[:, f, 128 * m:128 * m + 128], u[:, f, :],
                     start=(f == 0), stop=(f == 31), r=[("wd", f), ("u", f)], w=[pk])
            P.op("act", "activation", xb[:, m, :], pt[:, 0:GF], AF.Copy, r=[pk], w=[(bk, m)])
        rms(xb, bk)
        for m in range(8):
            x_, xk = xr.next()
            P.dma("sp", x_[:], xv[:, m, cols], w=[xk])
            t_, tk = tt.next()
            o_, ok = ot.next()
            P.op("dve", "scalar_tensor_tensor", t_[:], xb[:, m, :], g2[:, m:m + 1], rstd[:], ALU.mult, ALU.mult,
                 r=[(bk, m), "g2", "rstd"], w=[tk])
            P.op("pool", "tensor_tensor", o_[:], t_[:], x_[:], ALU.add, r=[tk, xk], w=[ok])
            P.dma("sp", ov[:, m, cols], o_[:], r=[ok])
    c.end()


class DR:
    def __init__(self, c):
        self.c = c
        self.d = {}
        self.in_names = []
        spec = {"xT": ([D, T], F32), "pos": ([1, T], I32), "fcs": ([128, 2], F32), "identf": ([4, 4], F32),
                "ident": ([128, 128], BF16), "tri": ([128, 128], BF16), "trif": ([128, 128], F32),
                "eh": ([32, NT_ALL * 128], BF16), "gmask": ([128, 16, 32], F32), "mask2": ([128, 16, 32], F32),
                "mcol": ([64, 4, NSEG], F32)}
        for L in range(DEPTH):
            spec.update({f"wA{L}": ([D, NCOLA], F32), f"gpre{L}": ([128, 8], F32), f"gbias{L}": ([4, 2], F32),
                         f"gml{L}": ([128, 4], F32), f"w_out{L}": ([D, D], F32), f"gpost{L}": ([128, 8], F32),
                         f"w_up{L}": ([D, DFF], F32), f"w_down{L}": ([DFF, D], F32),
                         f"g1_{L}": ([128, 8], F32), f"g2_{L}": ([128, 8], F32)})
        self.spec_in = spec
        self.spec_scr = {
            "qT": ([512, T], BF16), **{f"kT_src{i}": ([128, T], BF16) for i in range(4)}, **{f"kT_all{i}": ([512, T], BF16) for i in range(4)},
            **{f"vsrc{i}": ([128, 2, 16, 65], BF16) for i in range(4)},
            **{f"v_all{i}": ([512, 2 * 16 * 65], BF16) for i in range(4)},
            "km_src": ([512, 8], BF16), "km_all": ([2048, 8], BF16),
            "mqT": ([256, T], BF16), "mkT": ([256, T], BF16), "mvo": ([128, 16, 4, 129], BF16),
            "sgT": ([512, T], F32), "bT": ([4, T], F32), "igo": ([128, 16, 4], F32), "bo": ([128, 16, 4], F32),
            "cloc_src": ([64, 2064], F32), "cloc_all": ([256, 2064], F32),
            "blast_src": ([1, 16], F32), "blast_all": ([4, 16], F32),
            "mixT": ([D, T], BF16), "x1T": ([D, T], F32), "x2T": ([D, T], F32)}

    def __getitem__(self, name):
        if name not in self.d:
            if name in self.spec_in:
                shape, dt = self.spec_in[name]
                self.d[name] = self.c.din(name, shape, dt)
                self.in_names.append(name)
            else:
                shape, dt = self.spec_scr[name]
                self.d[name] = self.c.dscr(name, shape, dt)
        return self.d[name]


def build_fused(n_layers=DEPTH, stop_after=None):
    c = Ctx()
    dr = DR(c)
    out = c.dout("outT", [D, T], F32)
    c.dr = dr
    x_in = dr["xT"]
    for L in range(n_layers):
        last = (L == n_layers - 1)
        phase_A(c, dr, L, x_in)
        if stop_after in ("A", f"{L}:A"):
            break
        phase_CC(c, dr)
        if stop_after in ("CC", f"{L}:CC"):
            break
        phase_B(c, dr, L)
        if stop_after in ("B", f"{L}:B"):
            break
        phase_O(c, dr, L, x_in, dr["x1T"])
        if stop_after in ("O", f"{L}:O"):
            break
        phase_F(c, dr, L, dr["x1T"], out if last else dr["x2T"])
        x_in = dr["x2T"]
    nc = c.finish()
    nc._in_names = list(dr.in_names)
    return nc


def rope_consts():
    inv = ROPE_THETA ** (-np.arange(0, 16, 2, dtype=np.float32) / 16.0)
    f = (inv.astype(np.float64) / (2 * np.pi)).astype(np.float32)
    fc = np.zeros((128, 2), np.float32)
    for hh in range(2):
        for d in range(16):
            p = 64 * hh + d
            fc[p, 0] = f[d % 8]
            fc[p, 1] = -f[d % 8] if d < 8 else f[d % 8]
    return fc


def rope_perm():
    perm = np.arange(512)
    for h in range(8):
        for d in range(16):
            perm[64 * h + d] = 64 * h + (d + 8 if d < 8 else d - 8)
    return perm


def token_index(j):
    return np.concatenate([np.arange(g * G, (g + 1) * G) for g in core_groups(j)])


def core_consts(j):
    gs = core_groups(j)
    eh = np.zeros((32, NT_ALL * 128), dtype=NPBF)
    gmask = np.zeros((16, 32), np.float32)
    mask2 = np.zeros((16, 32), np.float32)
    mcol = np.zeros((4, NSEG), np.float32)
    for l in range(4):
        gq = gs[l]
        tiles = list(range(4 * gq, 4 * gq + 4)) + list(range(0, NT_SLOT[l] - 4))
        for pos_, tl in enumerate(tiles):
            p0 = (NT_OFF[l] + pos_) * 128
            if pos_ >= 4 and tl >= 4 * gq:
                eh[31, p0:p0 + 128] = 1
            else:
                eh[tl // 2, p0:p0 + 128] = 1
        for qt in range(4):
            own = 2 * gq + qt // 2
            gmask[4 * l + qt, own] = 1e30
            gmask[4 * l + qt, own + 1:] = -1e30
            mask2[4 * l + qt, own + 1:] = NEG
        mcol[l, :gq] = 1.0
    return dict(eh=eh,
                gmask=np.ascontiguousarray(np.broadcast_to(gmask[None], (128, 16, 32))),
                mask2=np.ascontiguousarray(np.broadcast_to(mask2[None], (128, 16, 32))),
                mcol=np.ascontiguousarray(np.broadcast_to(mcol[None], (64, 4, NSEG))))


def _pk(g):
    return np.ascontiguousarray(np.asarray(g, dtype=np.float32).reshape(8, 128).T)


_NC = {}


def make_inputs(x, positions, g_mix_pre, w_in, b_igate, b_fgate, g_mlstm_out, w_out,
                g_mix_post, g_mlp_pre, w_up, w_down, g_mlp_post):
    x = np.asarray(x, dtype=np.float32)
    positions = np.asarray(positions).astype(np.int32)
    perm = rope_perm()
    ident = np.eye(128, dtype=np.float32)
    tri = (np.arange(128)[:, None] <= np.arange(128)[None, :]).astype(np.float32)
    shared = dict(fcs=rope_consts(), identf=np.eye(4, dtype=np.float32), ident=ident.astype(NPBF),
                  tri=tri.astype(NPBF), trif=tri)
    for L in range(DEPTH):
        wi = np.asarray(w_in[L], dtype=np.float32)
        shared[f"wA{L}"] = np.ascontiguousarray(np.concatenate([wi, wi[:, 0:512][:, perm], wi[:, 512:1024][:, perm]], axis=1))
        shared[f"gpre{L}"] = _pk(g_mix_pre[L])
        shared[f"gbias{L}"] = np.ascontiguousarray(np.stack([np.asarray(b_igate[L]), np.asarray(b_fgate[L])], axis=1).astype(np.float32))
        shared[f"gml{L}"] = np.ascontiguousarray(np.asarray(g_mlstm_out[L], dtype=np.float32).reshape(4, 128).T)
        shared[f"w_out{L}"] = np.ascontiguousarray(np.asarray(w_out[L], dtype=np.float32))
        shared[f"gpost{L}"] = _pk(g_mix_post[L])
        shared[f"w_up{L}"] = np.ascontiguousarray(np.asarray(w_up[L], dtype=np.float32))
        shared[f"w_down{L}"] = np.ascontiguousarray(np.asarray(w_down[L], dtype=np.float32))
        shared[f"g1_{L}"] = _pk(g_mlp_pre[L])
        shared[f"g2_{L}"] = _pk(g_mlp_post[L])
    in_maps = []
    for core in range(NCORE):
        b, j = core // 4, core % 4
        ti = token_index(j)
        m = dict(shared)
        m.update(core_consts(j))
        m["xT"] = np.ascontiguousarray(x[b, ti, :].T)
        m["pos"] = np.ascontiguousarray(positions[b, ti][None, :])
        in_maps.append(m)
    return in_maps


N_LAUNCH_LAYERS = 1


def kernel(**inputs):
    in_maps = make_inputs(**inputs)
    key = ("nc", N_LAUNCH_LAYERS)
    if key not in _NC:
        _NC[key] = build_fused(n_layers=N_LAUNCH_LAYERS)
    nc = _NC[key]
    names = nc._in_names
    xs = [m["xT"] for m in in_maps]
    for L0 in range(0, DEPTH, N_LAUNCH_LAYERS):
        maps = []
        for core, m in enumerate(in_maps):
            d = {}
            for k in names:
                if k == "xT":
                    d[k] = xs[core]
                elif k[-1].isdigit() and k[:-1] in ("wA", "gpre", "gbias", "gml", "w_out", "gpost", "w_up", "w_down", "g1_", "g2_"):
                    d[k] = m[k[:-1] + str(int(k[-1]) + L0)]
                else:
                    d[k] = m[k]
            maps.append(d)
        res = run_bass_kernel_spmd(nc, maps, core_ids=list(range(NCORE)))
        xs = [np.ascontiguousarray(np.asarray(res.results[core]["outT"])) for core in range(NCORE)]
    out = np.zeros((NB, S, D), dtype=np.float32)
    for core in range(NCORE):
        b, j = core // 4, core % 4
        out[b, token_index(j), :] = xs[core].T
    return out
```
